# Optimizing a Trainium2 kernel written in Bass

```python
import math
import jax, jax.numpy as jnp
from jax import lax
import numpy as np

D_MODEL = 4096
BATCH = 4
SEQ = 4096
DEPTH = 4

GRID_W = 64
CTX_LEN = 256
DN_HEADS = 16
DN_HEAD_DIM = 128
DN_W = DN_HEADS * DN_HEAD_DIM
CONF_W = 2048
MIX_W = DN_W + CONF_W
SHORT_CONV = 5
DN_CHUNK = 64
CONF_K = 31
IN_W = 4 * DN_W + 4 * DN_HEADS + 2 * CONF_W
CONF_OFF = 4 * DN_W + 4 * DN_HEADS
N_EXPERTS = 16
EXPERT_FF = 256
CAP_FACTOR = 2
EPS = 1e-6

kernel_name = "hymba_deltanet_conformer_ecmoe_dit"


def rmsnorm(x):
    xf = x.astype(jnp.float32)
    return (xf * lax.rsqrt(jnp.mean(xf * xf, axis=-1, keepdims=True) + EPS)).astype(x.dtype)


def layernorm(x, g, b):
    xf = x.astype(jnp.float32)
    mu = jnp.mean(xf, axis=-1, keepdims=True)
    var = jnp.mean(jnp.square(xf - mu), axis=-1, keepdims=True)
    return ((xf - mu) * lax.rsqrt(var + EPS) * g + b).astype(x.dtype)


def l2norm(x):
    return x * lax.rsqrt(jnp.sum(x * x, axis=-1, keepdims=True) + EPS)


def adaln(cond, w, b):
    m = (jax.nn.silu(cond) @ w + b).reshape(-1, 6, D_MODEL)
    return jnp.moveaxis(m, 1, 0)[:, :, None, :]


def modulate(h, shift, scale):
    return h * (1.0 + scale) + shift


def depthwise_conv1d(x, w):
    k, ch = w.shape
    pad = k // 2
    return lax.conv_general_dilated(x, w.reshape(k, 1, ch), (1,), ((pad, pad),),
                                    dimension_numbers=("NWC", "WIO", "NWC"), feature_group_count=ch)


def depthwise_conv_grid(x, w, axis):
    k, ch = w.shape
    pad = k // 2
    if axis == 1:
        kern, padding = w.reshape(k, 1, 1, ch), ((pad, pad), (0, 0))
    else:
        kern, padding = w.reshape(1, k, 1, ch), ((0, 0), (pad, pad))
    return lax.conv_general_dilated(x, kern, (1, 1), padding,
                                    dimension_numbers=("NHWC", "HWIO", "NHWC"), feature_group_count=ch)


def chunk_gated_delta(q, k, v, g, beta, s0):
    f32 = jnp.float32
    bsz, t_len, h, dk = q.shape
    dv = v.shape[-1]
    n = t_len // DN_CHUNK
    q = l2norm(q.astype(f32)) * (dk ** -0.5)
    k = l2norm(k.astype(f32))

    def to_chunks(t):
        return jnp.moveaxis(t.reshape((bsz, n, DN_CHUNK, h) + t.shape[3:]), 3, 1)

    qc, kc, vc = to_chunks(q), to_chunks(k), to_chunks(v.astype(f32))
    gc = jnp.cumsum(to_chunks(g.astype(f32)), axis=-1)
    bc = to_chunks(beta.astype(f32))
    tri = jnp.tril(jnp.ones((DN_CHUNK, DN_CHUNK), dtype=bool))
    strict = jnp.tril(jnp.ones((DN_CHUNK, DN_CHUNK), dtype=bool), k=-1)
    decay = jnp.exp(jnp.where(tri, gc[..., :, None] - gc[..., None, :], -jnp.inf))
    a_mat = jnp.where(strict, jnp.einsum("bhnid,bhnjd->bhnij", kc, kc) * decay * bc[..., :, None], 0.0)
    lhs = a_mat + jnp.eye(DN_CHUNK, dtype=f32)
    rhs = jnp.concatenate([vc * bc[..., None], kc * (bc * jnp.exp(gc))[..., None]], axis=-1)
    sol = lax.linalg.triangular_solve(lhs, rhs, left_side=True, lower=True, unit_diagonal=True)
    u, w = sol[..., :dv], sol[..., dv:]
    qk = jnp.where(tri, jnp.einsum("bhnid,bhnjd->bhnij", qc, kc) * decay, 0.0)
    q_dec = qc * jnp.exp(gc)[..., None]
    g_last = gc[..., -1]
    k_dec = kc * jnp.exp(g_last[..., None] - gc)[..., None]
    xs = tuple(jnp.moveaxis(t, 2, 0) for t in (w, u, qk, q_dec, k_dec, g_last))

    def step(s, inp):
        w_i, u_i, qk_i, qd_i, kd_i, gl_i = inp
        v_new = u_i - jnp.einsum("bhck,bhkv->bhcv", w_i, s)
        o_i = jnp.einsum("bhck,bhkv->bhcv", qd_i, s) + jnp.einsum("bhij,bhjv->bhiv", qk_i, v_new)
        s = s * jnp.exp(gl_i)[..., None, None] + jnp.einsum("bhck,bhcv->bhkv", kd_i, v_new)
        return s, o_i

    s_fin, o = lax.scan(step, s0.astype(f32), xs)
    o = jnp.moveaxis(jnp.moveaxis(o, 0, 2), 1, 3).reshape(bsz, t_len, h, dv)
    return o, s_fin


def deltanet_group(p, conv_w, a_log, dt_bias, norm_w, s0):
    bsz, t_len, _ = p.shape
    qkv = jax.nn.silu(depthwise_conv1d(p[..., :3 * DN_W], conv_w))
    q, k, v = [t.reshape(bsz, t_len, DN_HEADS, DN_HEAD_DIM) for t in jnp.split(qkv, 3, axis=-1)]
    z = p[..., 3 * DN_W:4 * DN_W].reshape(bsz, t_len, DN_HEADS, DN_HEAD_DIM)
    ba = p[..., 4 * DN_W:CONF_OFF].astype(jnp.float32).reshape(bsz, t_len, 4, DN_HEADS)
    beta = jax.nn.sigmoid(ba[:, :, :2])
    g = -jnp.exp(a_log.astype(jnp.float32)) * jax.nn.softplus(ba[:, :, 2:] + dt_bias.astype(jnp.float32))
    flip = lambda t: jnp.flip(t, axis=1)
    o_f, s_f = chunk_gated_delta(q, k, v, g[:, :, 0], beta[:, :, 0], s0[0])
    o_b, s_b = chunk_gated_delta(flip(q), flip(k), flip(v), flip(g[:, :, 1]), flip(beta[:, :, 1]), s0[1])
    o = o_f + flip(o_b)
    o = rmsnorm(o) * norm_w.astype(jnp.float32) * jax.nn.silu(z.astype(jnp.float32))
    return o.reshape(bsz, t_len, DN_W).astype(p.dtype), jnp.stack([s_f, s_b])


def conformer_group(p, dw, dw_b, ln_g, ln_b, on_grid):
    a = p[..., CONF_OFF:CONF_OFF + CONF_W]
    gate = p[..., CONF_OFF + CONF_W:]
    u = a * jax.nn.sigmoid(gate)
    if on_grid:
        bsz, t_len, ch = u.shape
        rows = t_len // GRID_W
        ug = u.reshape(bsz, rows, GRID_W, ch)
        half = ch // 2
        y_h = depthwise_conv_grid(ug[..., :half], dw[:, :half], axis=2)
        y_v = depthwise_conv_grid(ug[..., half:], dw[:, half:], axis=1)
        y = jnp.concatenate([y_h, y_v], axis=-1).reshape(bsz, t_len, ch)
    else:
        y = depthwise_conv1d(u, dw)
    return jax.nn.silu(layernorm(y + dw_b, ln_g, ln_b))


def expert_choice_ffn(h, w_router, w_gate, w_up, w_down):
    bsz, t_len, _ = h.shape
    cap = (CAP_FACTOR * t_len) // N_EXPERTS
    aff = jax.nn.softmax(jnp.einsum("btd,de->bte", h.astype(jnp.float32), w_router.astype(jnp.float32)), axis=-1)
    g, idx = lax.top_k(jnp.swapaxes(aff, 1, 2), cap)
    bidx = jnp.arange(bsz)[:, None, None]
    xs = h[bidx, idx]
    a = jnp.einsum("becd,edf->becf", xs, w_gate)
    up = jnp.einsum("becd,edf->becf", xs, w_up)
    y = jnp.einsum("becf,efd->becd", jax.nn.silu(a) * up, w_down) * g[..., None].astype(h.dtype)
    return jnp.zeros_like(h).at[bidx, idx].add(y)


def setup_inputs(seed: int = 0) -> dict:
    key = jax.random.key(seed)
    ks = jax.random.split(key, 24)
    f32 = jnp.float32
    nrm = lambda k, shape, s: jax.random.normal(k, shape, f32) * s
    dt = jnp.exp(jax.random.uniform(ks[9], (DEPTH, 2, DN_HEADS), f32, math.log(1e-3), math.log(1e-1)))
    return {
        "x": nrm(ks[0], (BATCH, SEQ, D_MODEL), 1.0),
        "c": nrm(ks[1], (BATCH, D_MODEL), 1.0),
        "ctx": nrm(ks[2], (BATCH, CTX_LEN, D_MODEL), 1.0),
        "c_ctx": nrm(ks[3], (D_MODEL,), 1.0),
        "w_ada": nrm(ks[4], (DEPTH, D_MODEL, 6 * D_MODEL), 0.5 * D_MODEL ** -0.5),
        "b_ada": nrm(ks[5], (DEPTH, 6 * D_MODEL), 0.02),
        "w_in": nrm(ks[6], (DEPTH, D_MODEL, IN_W), D_MODEL ** -0.5),
        "conv_qkv": nrm(ks[7], (DEPTH, SHORT_CONV, 3 * DN_W), SHORT_CONV ** -0.5),
        "a_log": jnp.log(jax.random.uniform(ks[8], (DEPTH, 2, DN_HEADS), f32, 1.0, 16.0)),
        "dt_bias": dt + jnp.log(-jnp.expm1(-dt)),
        "dn_norm": 1.0 + nrm(ks[10], (DEPTH, DN_HEAD_DIM), 0.02),
        "conf_dw": nrm(ks[11], (DEPTH, CONF_K, CONF_W), CONF_K ** -0.5),
        "conf_dw_b": nrm(ks[12], (DEPTH, CONF_W), 0.02),
        "conf_ln_g": 1.0 + nrm(ks[13], (DEPTH, CONF_W), 0.02),
        "conf_ln_b": nrm(ks[14], (DEPTH, CONF_W), 0.02),
        "w_out": nrm(ks[15], (DEPTH, MIX_W, D_MODEL), MIX_W ** -0.5),
        "w_router": nrm(ks[16], (DEPTH, D_MODEL, N_EXPERTS), D_MODEL ** -0.5),
        "w_gate_e": nrm(ks[17], (DEPTH, N_EXPERTS, D_MODEL, EXPERT_FF), D_MODEL ** -0.5),
        "w_up_e": nrm(ks[18], (DEPTH, N_EXPERTS, D_MODEL, EXPERT_FF), D_MODEL ** -0.5),
        "w_down_e": nrm(ks[19], (DEPTH, N_EXPERTS, EXPERT_FF, D_MODEL), EXPERT_FF ** -0.5),
        "final_norm": 1.0 + nrm(ks[20], (D_MODEL,), 0.02),
    }


def reference(x, c, ctx, c_ctx, w_ada, b_ada, w_in, conv_qkv, a_log, dt_bias, dn_norm, conf_dw,
              conf_dw_b, conf_ln_g, conf_ln_b, w_out, w_router, w_gate_e, w_up_e, w_down_e, final_norm):
    bsz = x.shape[0]
    x_lat, x_ctx = x, ctx
    zero_state = jnp.zeros((2, bsz, DN_HEADS, DN_HEAD_DIM, DN_HEAD_DIM), jnp.float32)
    for l in range(DEPTH):
        last = l == DEPTH - 1
        ml = adaln(c, w_ada[l], b_ada[l])
        mc = adaln(c_ctx[None], w_ada[l], b_ada[l])
        p_lat = modulate(rmsnorm(x_lat), ml[0], ml[1]) @ w_in[l]
        p_ctx = modulate(rmsnorm(x_ctx), mc[0], mc[1]) @ w_in[l]
        dn_ctx, ctx_states = deltanet_group(p_ctx, conv_qkv[l], a_log[l], dt_bias[l], dn_norm[l], zero_state)
        dn_lat, _ = deltanet_group(p_lat, conv_qkv[l], a_log[l], dt_bias[l], dn_norm[l], ctx_states)
        cf_lat = conformer_group(p_lat, conf_dw[l], conf_dw_b[l], conf_ln_g[l], conf_ln_b[l], True)
        y_lat = jnp.concatenate([dn_lat, cf_lat], axis=-1) @ w_out[l]
        x_lat = x_lat + ml[2] * y_lat
        h_lat = modulate(rmsnorm(x_lat), ml[3], ml[4])
        x_lat = x_lat + ml[5] * expert_choice_ffn(h_lat, w_router[l], w_gate_e[l], w_up_e[l], w_down_e[l])
        if not last:
            cf_ctx = conformer_group(p_ctx, conf_dw[l], conf_dw_b[l], conf_ln_g[l], conf_ln_b[l], False)
            y_ctx = jnp.concatenate([dn_ctx, cf_ctx], axis=-1) @ w_out[l]
            x_ctx = x_ctx + mc[2] * y_ctx
            h_ctx = modulate(rmsnorm(x_ctx), mc[3], mc[4])
            x_ctx = x_ctx + mc[5] * expert_choice_ffn(h_ctx, w_router[l], w_gate_e[l], w_up_e[l], w_down_e[l])
    return rmsnorm(x_lat) * final_norm
```

```python
import numpy as np
from contextlib import ExitStack
import concourse.bass as bass
import concourse.mybir as mybir
from concourse.bass_utils import run_bass_kernel_spmd

F32 = mybir.dt.float32
BF16 = mybir.dt.bfloat16
AF = mybir.ActivationFunctionType
ALU = mybir.AluOpType
AX = mybir.AxisListType
EPS = 1e-6
NCORES = 8

FULL = dict(D=4096, TL=4096, TC=256, H=16, CW=2048, GW=64, E=16, FF=256, L=4, B=4)


def derive(c):
    c = dict(c)
    c["KC"] = c["D"] // 128
    c["TA"] = c["TL"] + c["TC"]
    c["NTL"] = c["TL"] // 128
    c["NTC"] = c["TC"] // 128
    c["NTA"] = c["TA"] // 128
    c["DNW"] = c["H"] * 128
    c["CT"] = c["CW"] // 128
    c["ROWS"] = c["TL"] // c["GW"]
    c["MIXW"] = c["DNW"] + c["CW"]
    c["KCM"] = c["MIXW"] // 128
    c["INW"] = 4 * c["DNW"] + 4 * c["H"] + 2 * c["CW"]
    c["OFF_Z"] = 3 * c["DNW"]
    c["OFF_A"] = 4 * c["DNW"]
    c["OFF_G"] = 4 * c["DNW"] + c["CW"]
    c["OFF_BA"] = 4 * c["DNW"] + 2 * c["CW"]
    c["NBIN"] = (c["INW"] + 127) // 128
    c["FT"] = c["FF"] // 128
    c["EF"] = c["E"] * c["FF"]
    c["KCE"] = c["EF"] // 128
    c["NSH"] = 6 * c["D"] // NCORES
    c["CAPL"] = 2 * c["TL"] // c["E"]
    c["CAPC"] = 2 * c["TC"] // c["E"]
    szs = [c["NBIN"] * 128 * c["D"], c["KC"] * 128 * c["MIXW"], 2 * c["KCE"] * 128 * c["D"],
           c["KC"] * 128 * c["EF"]]
    offs = [0]
    for s in szs:
        offs.append(offs[-1] + s)
    c["WOFF"] = offs
    c["WFLAT"] = (offs[-1] + 2047) // 2048 * 2048
    return c


class Sem:
    def __init__(self, h):
        self.h = h
        self.count = 0


class Tok:
    __slots__ = ("w", "r", "ds", "excl")

    def __init__(self, excl=False):
        self.w = {}
        self.r = {}
        self.ds = None
        self.excl = excl


class Eng:
    def __init__(self, name, eng, sem):
        self.name = name
        self.eng = eng
        self.sem = sem
        self.waited = {}


class KB:
    def __init__(self, cfg):
        self.c = cfg
        self.nc = bass.Bass("TRN2", target_bir_lowering=False)
        nc = self.nc
        self.gstack = ExitStack()
        self.pstack = None
        mk = lambda n: Sem(self.gstack.enter_context(nc.semaphore(n)))
        self.PE = Eng("pe", nc.tensor, mk("s_pe"))
        self.DVE = Eng("dve", nc.vector, mk("s_dve"))
        self.ACT = Eng("act", nc.scalar, mk("s_act"))
        self.POOL = Eng("pool", nc.gpsimd, mk("s_pool"))
        self.SP = Eng("sp", nc.sync, mk("s_sp"))
        self.engs = [self.PE, self.DVE, self.ACT, self.POOL, self.SP]
        self.dpool = [mk(f"s_d{i}") for i in range(40)]
        self.dfree = list(self.dpool)
        self.ptoks = []
        self.banks = []
        for i in range(8):
            t = self.gstack.enter_context(nc.psum_tensor(f"ps{i}", [128, 512], F32))
            self.banks.append((t, Tok(excl=True)))
        self.bi = 0
        self.nid = 0

    def sb(self, shape, dt=F32, glob=False):
        self.nid += 1
        st = self.gstack if glob else self.pstack
        return st.enter_context(self.nc.sbuf_tensor(f"t{self.nid}", list(shape), dt))

    def tok(self, glob=False):
        t = Tok()
        if not glob:
            self.ptoks.append(t)
        return t

    def bank(self):
        b = self.banks[self.bi % 8]
        self.bi += 1
        return b

    def begin(self):
        self.pstack = ExitStack()
        self.ptoks = []

    def end(self):
        self.barrier()
        for t in self.ptoks:
            if t.ds is not None:
                self.dfree.append(t.ds)
                t.ds = None
        self.ptoks = []
        self.pstack.close()
        self.pstack = None

    def _wait(self, E, need):
        for s, v in need.items():
            if E.waited.get(s, 0) < v:
                E.eng.wait_ge(s.h, v)
                E.waited[s] = v

    def op(self, E, fn, outs=(), ins=()):
        need = {}

        def add(d, skip_same):
            for s, v in d.items():
                if s is E.sem and (skip_same or E is self.PE):
                    continue
                if need.get(s, 0) < v:
                    need[s] = v
        for t in ins:
            add(t.w, False)
            if t.excl:
                add(t.r, True)
        for t in outs:
            add(t.r, False)
            add(t.w, True)
        self._wait(E, need)
        ins_ = fn(E.eng)
        E.sem.count += 1
        ins_.then_inc(E.sem.h, 1)
        v = E.sem.count
        for t in ins:
            t.r[E.sem] = v
        for t in outs:
            t.w = {E.sem: v}
            t.r = {}
        return ins_

    def dma(self, out_ap, in_ap, wtok=None, rtok=None, extra_r=()):
        Q = self.SP
        need = {}

        def add(d):
            for s, v in d.items():
                if need.get(s, 0) < v:
                    need[s] = v
        if rtok is not None:
            add(rtok.w)
        for t in extra_r:
            add(t.w)
        if wtok is not None:
            add(wtok.w)
            add(wtok.r)
        self._wait(Q, need)
        t = wtok if wtok is not None else rtok
        if t.ds is None:
            t.ds = self.dfree.pop()
        ds = t.ds
        ds.count += 16
        Q.eng.dma_start(out=out_ap, in_=in_ap).then_inc(ds.h, 16)
        if wtok is not None:
            wtok.w = {ds: ds.count}
            wtok.r = {}
        if rtok is not None:
            rtok.r[ds] = ds.count

    def barrier(self):
        sems = [e.sem for e in self.engs] + self.dpool
        for E in self.engs:
            for s in sems:
                if s.count > 0 and E.waited.get(s, 0) < s.count:
                    E.eng.wait_ge(s.h, s.count)
                    E.waited[s] = s.count


def build(cfg):
    c = derive(cfg)
    kb = KB(c)
    nc = kb.nc
    PE, DVE, ACT, POOL = kb.PE, kb.DVE, kb.ACT, kb.POOL
    D, KC, TL, TC, TA, NTL, NTC, NTA = (c[k] for k in ("D", "KC", "TL", "TC", "TA", "NTL", "NTC", "NTA"))
    H, DNW, CW, CT, GW, ROWS, MIXW, KCM = (c[k] for k in ("H", "DNW", "CW", "CT", "GW", "ROWS", "MIXW", "KCM"))
    E_, FF, FT, EF, KCE, L, NSH, NBIN = (c[k] for k in ("E", "FF", "FT", "EF", "KCE", "L", "NSH", "NBIN"))
    WFLAT, WOFF = c["WFLAT"], c["WOFF"]

    def din(name, shape, dt=F32):
        return nc.dram_tensor(name, list(shape), dt, kind="ExternalInput").ap()

    def dint(name, shape, dt=F32):
        return nc.dram_tensor(name, list(shape), dt, kind="Internal").ap()

    xT_in = din("xT", [D, TA])
    modsel = din("modsel", [128, L * 6 * KC * 2])
    wall_in = din("wall", [L * (WFLAT // 2048), 2048])
    convw = din("convw", [128, L * 3 * H * 5])
    alog_b = din("alog_b", [128, L * 2 * H])
    dtb_b = din("dtb_b", [128, L * 2 * H])
    dnn = din("dnn", [128, L])
    cdw = din("cdw", [128, L * CT * 31])
    cvec = din("cvec", [128, L * 3 * CT])
    wrt = din("wrt", [128, L * KC * 16])
    fnorm = din("fnorm", [128, KC])
    consts = din("consts", [128, 6 * 128])
    selmat = din("selmat", [16, 16 * 128])
    outT = nc.dram_tensor("outT", [D, TL], F32, kind="ExternalOutput").ap()

    xT = dint("xres", [D, TA])
    hT = dint("hT", [NTA, 128, KC, 128], BF16)
    pT = dint("pT", [NBIN * 128, TA])
    qTd = dint("qTd", [H, 128, TA])
    kTd = dint("kTd", [H, 128, TA])
    ktokd = dint("ktokd", [H, NTA, 128, 128])
    vtokd = dint("vtokd", [H, NTA, 128, 128])
    catT = dint("catT", [NTA, 128, KCM, 128], BF16)
    yTd = dint("yTd", [CW, TA])
    actT = dint("actT", [NTA, 128, KCE, 128], BF16)
    lnst = dint("lnst", [2, 128, TA])

    def wview(l, idx, nb, kck):
        fl = wall_in[l * (WFLAT // 2048):(l + 1) * (WFLAT // 2048), :].rearrange("r c -> (r c)")
        return fl[WOFF[idx]:WOFF[idx] + nb * 128 * kck * 128].rearrange("(n k) -> n k", k=kck * 128)

    cst = kb.sb([128, 6 * 128], glob=True)
    cst_t = kb.tok(True)
    ident, ones, m_le, m_ge, m_lt, m_gt = (cst[:, i * 128:(i + 1) * 128] for i in range(6))
    selm = kb.sb([16, 16 * 128], glob=True)
    modT = kb.sb([128, L, 6, KC, 2], glob=True)
    mod_t = kb.tok(True)
    col = kb.sb([128, 4], glob=True)
    col_t = kb.tok(True)
    affT = kb.sb([16, TA], glob=True)
    aff_t = kb.tok(True)
    gwT = affT
    gw_t = aff_t
    smalls = kb.sb([128, L * 3 * H * 5 + 2 * L * 2 * H + L + L * CT * 31 + L * 3 * CT + L * KC * 16 + KC], glob=True)
    sm_t = kb.tok(True)
    o = 0
    convw_s = smalls[:, o:o + L * 3 * H * 5]; o += L * 3 * H * 5
    alog_s = smalls[:, o:o + L * 2 * H]; o += L * 2 * H
    dtb_s = smalls[:, o:o + L * 2 * H]; o += L * 2 * H
    dnn_s = smalls[:, o:o + L]; o += L
    cdw_s = smalls[:, o:o + L * CT * 31]; o += L * CT * 31
    cvec_s = smalls[:, o:o + L * 3 * CT]; o += L * 3 * CT
    wrt_s = smalls[:, o:o + L * KC * 16]; o += L * KC * 16
    fn_s = smalls[:, o:o + KC]; o += KC
    eps_c, one_c, zero_c = col[:, 0:1], col[:, 1:2], col[:, 2:3]

    kb.dma(cst[:], consts[:, :], wtok=cst_t)
    kb.dma(selm[:], selmat[:, :], wtok=cst_t)
    for dst, src in ((convw_s, convw), (alog_s, alog_b), (dtb_s, dtb_b), (dnn_s, dnn), (cdw_s, cdw),
                     (cvec_s, cvec), (wrt_s, wrt), (fn_s, fnorm)):
        kb.dma(dst, src[:, :], wtok=sm_t)
    kb.op(DVE, lambda e: e.memset(col[:, 0:1], EPS), outs=[col_t])
    kb.op(DVE, lambda e: e.memset(col[:, 1:2], 1.0), outs=[col_t])
    kb.op(DVE, lambda e: e.memset(col[:, 2:3], 0.0), outs=[col_t])
    kb.op(DVE, lambda e: e.memset(col[:, 3:4], 0.5), outs=[col_t])

    kb.begin()
    cp_t = kb.tok()
    xv_i = xT_in.rearrange("(a p) t -> p a t", p=128)
    xv_o = xT.rearrange("(a p) t -> p a t", p=128)
    for a in range(KC):
        kb.dma(xv_o[:, a, :], xv_i[:, a, :], wtok=cp_t)
    kb.dma(modT[:].rearrange("p l a k t -> p (l a k t)"), modsel[:, :], wtok=mod_t)
    for l in range(L):
        for j6 in (1, 4):
            kb.op(DVE, lambda e, l=l, j6=j6: e.tensor_scalar(
                out=modT[:, l, j6].rearrange("p k t -> p (k t)"), in0=modT[:, l, j6].rearrange("p k t -> p (k t)"),
                scalar1=1.0, scalar2=None, op0=ALU.add), outs=[mod_t], ins=[mod_t])
    kb.end()
    cc_t = [kb.tok(True) for _ in range(L + 1)]

    def mcol(l, j6, kc, is_ctx):
        return modT[:, l, j6, kc, (1 if is_ctx else 0):(2 if is_ctx else 1)]

    def norm_phase(scale_fn, shift_fn, dst_tiles=None, dst_f32=None, router_l=None, ntiles=NTA):
        kb.begin()
        xs = [(kb.sb([128, KC, 128]), kb.tok()) for _ in range(2)]
        sq = kb.sb([128, KC, 128]); sq_t = kb.tok()
        h32 = kb.sb([128, KC, 128]); h32_t = kb.tok()
        hbs = [(kb.sb([128, KC, 128], BF16), kb.tok()) for _ in range(2)]
        rs = kb.sb([128, 128]); rs_t = kb.tok()
        tmps = [(kb.sb([128, 128]), kb.tok()) for _ in range(2)]
        sm = kb.sb([128, 64]); sm_tk = kb.tok()
        xv = xT.rearrange("(k p) t -> p k t", p=128)
        for ti in range(ntiles):
            is_ctx = ti >= NTL
            x, x_t = xs[ti % 2]
            import os as _os
            KSUB = int(_os.environ.get("KSUB", "100"))
            kb.dma(x[:], xv[:, :, ti * 128:(ti + 1) * 128], wtok=x_t)
            if KSUB == 1: break
            kb.op(ACT, lambda e, x=x: e.square(out=sq[:], in_=x[:]), outs=[sq_t], ins=[x_t])
            if KSUB == 2: break
            bt, bt_t = kb.bank()
            for kc in range(KC):
                kb.op(PE, lambda e, bt=bt, kc=kc: e.matmul(bt[:, 0:128], ones, sq[:, kc, :], start=(kc == 0),
                                                            stop=(kc == KC - 1)), outs=[bt_t], ins=[sq_t, cst_t])
            if KSUB == 3: break
            kb.op(ACT, lambda e, bt=bt: e.activation(out=rs[:], in_=bt[:, 0:128], func=AF.Sqrt, bias=eps_c,
                                                     scale=1.0 / D), outs=[rs_t], ins=[bt_t, col_t])
            if KSUB == 4: break
            kb.op(DVE, lambda e: e.reciprocal(out=rs[:], in_=rs[:]), outs=[rs_t], ins=[rs_t])
            if KSUB == 5: break
            hb, hb_t = hbs[ti % 2]
            for kc in range(KC):
                tm, tm_t = tmps[kc % 2]
                kb.op(DVE, lambda e, tm=tm, x=x, kc=kc: e.tensor_tensor(out=tm[:], in0=x[:, kc, :], in1=rs[:],
                                                                         op=ALU.mult), outs=[tm_t], ins=[x_t, rs_t])
                sh = shift_fn(kc, is_ctx)
                kb.op(ACT, lambda e, tm=tm, kc=kc, sh=sh, is_ctx=is_ctx: e.activation(
                    out=h32[:, kc, :], in_=tm[:], func=AF.Identity, bias=(sh if sh is not None else zero_c),
                    scale=scale_fn(kc, is_ctx)), outs=[h32_t], ins=[tm_t, mod_t, sm_t, col_t])
            if KSUB == 6: break
            if dst_f32 is not None:
                kb.dma(dst_f32.rearrange("(k p) t -> p k t", p=128)[:, :, ti * 128:(ti + 1) * 128], h32[:], rtok=h32_t)
                continue
            kb.op(POOL, lambda e, hb=hb: e.tensor_copy(out=hb[:], in_=h32[:]), outs=[hb_t], ins=[h32_t])
            if KSUB == 7: break
            kb.dma(dst_tiles[ti], hb[:], rtok=hb_t)
            if KSUB == 8: break
            if router_l is not None:
                bt, bt_t = kb.bank()
                for kc in range(KC):
                    w0 = (router_l * KC + kc) * 16
                    kb.op(PE, lambda e, bt=bt, kc=kc, w0=w0: e.matmul(
                        bt[:, 0:16], h32[:, kc, :], wrt_s[:, w0:w0 + 16], start=(kc == 0), stop=(kc == KC - 1)),
                        outs=[bt_t], ins=[h32_t, sm_t])
                mx, nmx, ssum, ex, af = sm[:, 0:1], sm[:, 1:2], sm[:, 2:3], sm[:, 8:24], sm[:, 24:40]
                kb.op(DVE, lambda e, bt=bt: e.reduce_max(out=mx, in_=bt[:, 0:16], axis=AX.X), outs=[sm_tk], ins=[bt_t, sm_tk])
                kb.op(DVE, lambda e: e.tensor_scalar(out=nmx, in0=mx, scalar1=-1.0, scalar2=None, op0=ALU.mult),
                      outs=[sm_tk], ins=[sm_tk])
                kb.op(ACT, lambda e, bt=bt: e.activation(out=ex, in_=bt[:, 0:16], func=AF.Exp, bias=nmx, scale=1.0),
                      outs=[sm_tk], ins=[bt_t, sm_tk])
                kb.op(DVE, lambda e: e.reduce_sum(out=ssum, in_=ex, axis=AX.X), outs=[sm_tk], ins=[sm_tk])
                kb.op(DVE, lambda e: e.reciprocal(out=ssum, in_=ssum), outs=[sm_tk], ins=[sm_tk])
                kb.op(DVE, lambda e: e.tensor_scalar(out=af, in0=ex, scalar1=ssum, scalar2=None, op0=ALU.mult),
                      outs=[sm_tk], ins=[sm_tk])
                b2, b2_t = kb.bank()
                kb.op(PE, lambda e, b2=b2: e.transpose(out=b2[0:16, 0:128], in_=af, identity=ident),
                      outs=[b2_t], ins=[sm_tk, cst_t])
                kb.op(DVE, lambda e, b2=b2, ti=ti: e.tensor_copy(out=affT[:, ti * 128:(ti + 1) * 128], in_=b2[0:16, 0:128]),
                      outs=[aff_t], ins=[b2_t])
        kb.end()

    def groups_of(tiles):
        gs = []
        cur = []
        for t in tiles:
            if cur and (len(cur) == 4 or (t >= NTL) != (cur[0] >= NTL)):
                gs.append(cur); cur = []
            cur.append(t)
        if cur:
            gs.append(cur)
        return gs

    def supertiles():
        nst = (NTA + 6) // 7
        base, rem = NTA // nst, NTA % nst
        sts = []
        t = 0
        for i in range(nst):
            n = base + (1 if i < rem else 0)
            sts.append(list(range(t, t + n)))
            t += n
        return sts

    def linear_phase(src, kck, wv, nb, cc_tok, epilogue, pair=False, pre_super=None):
        kb.begin()
        mx_tiles = max(len(s) for s in supertiles())
        S = kb.sb([128, mx_tiles, kck, 128], BF16); S_t = kb.tok()
        wfs = [(kb.sb([128, kck * 128]), kb.tok()) for _ in range(2)]
        wbs = [(kb.sb([128, kck, 128], BF16), kb.tok()) for _ in range(4 if pair else 2)]
        ctx_ = dict(kb=kb)
        if epilogue.setup:
            epilogue.setup(ctx_)
        wi = 0
        for st in supertiles():
            t0, nt = st[0], len(st)
            kb.dma(S[:, 0:nt], src[t0:t0 + nt].rearrange("n p k t -> p n k t"), wtok=S_t)
            gs = groups_of(st)
            if pre_super:
                pre_super(ctx_, st, gs)
            step = 2 if pair else 1
            for b0 in range(0, nb, step):
                wbl = []
                for bb in range(step):
                    wf, wf_t = wfs[wi % 2]
                    wb, wb_t = wbs[wi % len(wbs)]
                    kb.dma(wf[:], wv[(b0 + bb) * 128:(b0 + bb + 1) * 128, :], wtok=wf_t, extra_r=[cc_tok])
                    ce = DVE if wi % 2 == 0 else POOL
                    kb.op(ce, lambda e, wb=wb, wf=wf: e.tensor_copy(out=wb[:].rearrange("p k n -> p (k n)"), in_=wf[:]),
                          outs=[wb_t], ins=[wf_t])
                    wbl.append((wb, wb_t))
                    wi += 1
                for g in gs:
                    g0 = g[0] - t0
                    ng = len(g)
                    bks = []
                    for (wb, wb_t) in wbl:
                        bt, bt_t = kb.bank()
                        for kc in range(kck):
                            kb.op(PE, lambda e, bt=bt, wb=wb, kc=kc, g0=g0, ng=ng: e.matmul(
                                bt[:, 0:ng * 128], wb[:, kc, :], S[:, g0:g0 + ng, kc, :], start=(kc == 0),
                                stop=(kc == kck - 1)), outs=[bt_t], ins=[wb_t, S_t])
                        bks.append((bt, bt_t))
                    epilogue(ctx_, b0, g, bks)
        kb.end()

    class Epi:
        def __init__(self, fn, setup=None):
            self.fn = fn
            self.setup = setup

        def __call__(self, *a):
            return self.fn(*a)

    def epi_store(dst):
        def setup(cx):
            cx["st"] = [(kb.sb([128, 512]), kb.tok()) for _ in range(3)]
            cx["i"] = 0

        def fn(cx, b0, g, bks):
            (bt, bt_t), = bks
            st, st_t = cx["st"][cx["i"] % 3]
            n = len(g) * 128
            eng = ACT if cx["i"] % 2 == 0 else DVE
            cx["i"] += 1
            if eng is ACT:
                kb.op(ACT, lambda e: e.activation(out=st[:, 0:n], in_=bt[:, 0:n], func=AF.Copy), outs=[st_t], ins=[bt_t])
            else:
                kb.op(DVE, lambda e: e.tensor_copy(out=st[:, 0:n], in_=bt[:, 0:n]), outs=[st_t], ins=[bt_t])
            kb.dma(dst[b0 * 128:(b0 + 1) * 128, g[0] * 128:g[0] * 128 + n], st[:, 0:n], rtok=st_t)
        return Epi(fn, setup)

    def epi_resid(l, j6):
        def setup(cx):
            cx["xo"] = [(kb.sb([128, 512]), kb.tok()) for _ in range(3)]
            cx["i"] = 0

        def fn(cx, b0, g, bks):
            (bt, bt_t), = bks
            xo, xo_t = cx["xo"][cx["i"] % 3]
            cx["i"] += 1
            n = len(g) * 128
            reg = xT[b0 * 128:(b0 + 1) * 128, g[0] * 128:g[0] * 128 + n]
            kb.dma(xo[:, 0:n], reg, wtok=xo_t)
            gc_ = mcol(l, j6, b0, g[0] >= NTL)
            kb.op(DVE, lambda e: e.scalar_tensor_tensor(out=xo[:, 0:n], in0=bt[:, 0:n], scalar=gc_, in1=xo[:, 0:n],
                                                        op0=ALU.mult, op1=ALU.add), outs=[xo_t], ins=[bt_t, xo_t, mod_t])
            kb.dma(reg, xo[:, 0:n], rtok=xo_t)
        return Epi(fn, setup)

    def epi_moe():
        def setup(cx):
            cx["gwb"] = kb.sb([128, 12 * 128]); cx["gwb_t"] = kb.tok()
            cx["sg"] = [(kb.sb([128, 512]), kb.tok()) for _ in range(2)]
            cx["ab"] = [(kb.sb([128, 512], BF16), kb.tok()) for _ in range(3)]
            cx["i"] = 0
            cx["cur_e"] = None

        def fn(cx, b0, g, bks):
            (ba_, ba_t), (bu, bu_t) = bks
            kidx = b0 // 2
            e_ = kidx // FT
            st = cx["st_tiles"]
            if cx["cur_e"] != (st[0], e_):
                cx["cur_e"] = (st[0], e_)
                for gg in cx["gs"]:
                    bt, bt_t = kb.bank()
                    n = len(gg) * 128
                    o0 = (gg[0] - st[0]) * 128
                    kb.op(PE, lambda e, bt=bt, n=n, gg=gg: e.matmul(
                        bt[:, 0:n], selm[0:16, e_ * 128:(e_ + 1) * 128], gwT[0:16, gg[0] * 128:gg[0] * 128 + n],
                        start=True, stop=True), outs=[bt_t], ins=[gw_t, cst_t])
                    kb.op(ACT, lambda e, bt=bt, n=n, o0=o0: e.activation(out=cx["gwb"][:, o0:o0 + n], in_=bt[:, 0:n],
                                                                          func=AF.Copy), outs=[cx["gwb_t"]], ins=[bt_t])
            n = len(g) * 128
            o0 = (g[0] - st[0]) * 128
            sg, sg_t = cx["sg"][cx["i"] % 2]
            ab, ab_t = cx["ab"][cx["i"] % 3]
            cx["i"] += 1
            kb.op(ACT, lambda e: e.activation(out=sg[:, 0:n], in_=ba_[:, 0:n], func=AF.Silu), outs=[sg_t], ins=[ba_t])
            kb.op(DVE, lambda e: e.tensor_tensor(out=sg[:, 0:n], in0=sg[:, 0:n], in1=bu[:, 0:n], op=ALU.mult),
                  outs=[sg_t], ins=[sg_t, bu_t])
            kb.op(POOL, lambda e: e.tensor_tensor(out=ab[:, 0:n], in0=sg[:, 0:n], in1=cx["gwb"][:, o0:o0 + n], op=ALU.mult),
                  outs=[ab_t], ins=[sg_t, cx["gwb_t"]])
            kb.dma(actT[g[0]:g[0] + len(g), :, kidx, :].rearrange("n p t -> p n t"),
                   ab[:, 0:n].rearrange("p (n t) -> p n t", t=128), rtok=ab_t)

        def pre(cx, st, gs):
            cx["st_tiles"] = st
            cx["gs"] = gs
        return Epi(fn, setup), pre

    def gates_phase(l, gate_b, gate_g, gt_t):
        kb.begin()
        baT = kb.sb([4 * H, TA]); ba_t = kb.tok()
        kb.dma(baT[:], pT[c["OFF_BA"]:c["OFF_BA"] + 4 * H, :], wtok=ba_t)
        raw = kb.sb([128, NTA, 4 * H]); raw_t = kb.tok()
        for ti in range(NTA):
            bt, bt_t = kb.bank()
            kb.op(PE, lambda e, bt=bt, ti=ti: e.transpose(out=bt[:, 0:4 * H], in_=baT[:, ti * 128:(ti + 1) * 128],
                                                           identity=ident[0:4 * H, 0:4 * H]), outs=[bt_t], ins=[ba_t, cst_t])
            kb.op(DVE, lambda e, bt=bt, ti=ti: e.tensor_copy(out=raw[:, ti, :], in_=bt[:, 0:4 * H]), outs=[raw_t], ins=[bt_t])
        nega = kb.sb([128, 2 * H]); ng_t = kb.tok()
        kb.op(ACT, lambda e: e.activation(out=nega[:], in_=alog_s[:, l * 2 * H:(l + 1) * 2 * H], func=AF.Exp),
              outs=[ng_t], ins=[sm_t])
        kb.op(DVE, lambda e: e.tensor_scalar(out=nega[:], in0=nega[:], scalar1=-1.0, scalar2=None, op0=ALU.mult),
              outs=[ng_t], ins=[ng_t])
        w1 = kb.sb([128, 2 * H]); w1_t = kb.tok()
        w2 = kb.sb([128, 2 * H]); w2_t = kb.tok()
        w3 = kb.sb([128, 2 * H]); w3_t = kb.tok()
        for ti in range(NTA):
            kb.op(ACT, lambda e, ti=ti: e.activation(out=w1[:], in_=raw[:, ti, 0:2 * H], func=AF.Exp, scale=-1.0),
                  outs=[w1_t], ins=[raw_t])
            kb.op(DVE, lambda e: e.tensor_scalar(out=w1[:], in0=w1[:], scalar1=1.0, scalar2=None, op0=ALU.add),
                  outs=[w1_t], ins=[w1_t])
            kb.op(DVE, lambda e, ti=ti: e.reciprocal(out=gate_b[:, ti, :], in_=w1[:]), outs=[gt_t], ins=[w1_t])
            kb.op(DVE, lambda e, ti=ti: e.tensor_tensor(out=w2[:], in0=raw[:, ti, 2 * H:4 * H],
                                                        in1=dtb_s[:, l * 2 * H:(l + 1) * 2 * H], op=ALU.add),
                  outs=[w2_t], ins=[raw_t, sm_t])
            kb.op(DVE, lambda e: e.tensor_scalar(out=w3[:], in0=w2[:], scalar1=-1.0, scalar2=None, op0=ALU.mult),
                  outs=[w3_t], ins=[w2_t])
            kb.op(DVE, lambda e: e.tensor_tensor(out=w3[:], in0=w3[:], in1=w2[:], op=ALU.max),
                  outs=[w3_t], ins=[w2_t, w3_t])
            kb.op(ACT, lambda e: e.activation(out=w3[:], in_=w3[:], func=AF.Exp, scale=-1.0), outs=[w3_t], ins=[w3_t])
            kb.op(ACT, lambda e: e.activation(out=w3[:], in_=w3[:], func=AF.Ln, bias=one_c, scale=1.0),
                  outs=[w3_t], ins=[w3_t, col_t])
            kb.op(DVE, lambda e: e.scalar_tensor_tensor(out=w3[:], in0=w2[:], scalar=0.0, in1=w3[:], op0=ALU.max,
                                                        op1=ALU.add), outs=[w3_t], ins=[w2_t, w3_t])
            kb.op(DVE, lambda e, ti=ti: e.tensor_tensor(out=gate_g[:, ti, :], in0=w3[:], in1=nega[:], op=ALU.mult),
                  outs=[gt_t], ins=[w3_t, ng_t])
        kb.end()

    def qkv_phase(l):
        kb.begin()
        PADW = TL + 4 + TC + 4
        xps = [(kb.sb([128, PADW]), kb.tok()) for _ in range(2)]
        for xp, xp_t in xps:
            kb.op(POOL, lambda e, xp=xp: e.memset(xp[:], 0.0), outs=[xp_t])
        accs = [(kb.sb([128, TA]), kb.tok()) for _ in range(2)]
        sq = kb.sb([128, TA]); sq_t = kb.tok()
        rn = kb.sb([128, 512]); rn_t = kb.tok()
        sts = [(kb.sb([128, 4, 128]), kb.tok()) for _ in range(2)]
        u = 0
        for comp in range(3):
            for h in range(H):
                xp, xp_t = xps[u % 2]
                acc, acc_t = accs[u % 2]
                ce = DVE
                u += 1
                r0 = comp * DNW + h * 128
                kb.dma(xp[:, 2:2 + TL], pT[r0:r0 + 128, 0:TL], wtok=xp_t)
                kb.dma(xp[:, TL + 6:TL + 6 + TC], pT[r0:r0 + 128, TL:TA], wtok=xp_t)
                import os as _os
                QS = int(_os.environ.get("QSUB", "100"))
                if QS == 1: break
                w0 = ((l * 3 + comp) * H + h) * 5
                for (o_out, o_in, n) in ((0, 0, TL), (TL, TL + 4, TC)):
                    for j in range(5):
                        wj = convw_s[:, w0 + j:w0 + j + 1]
                        if j == 0:
                            kb.op(ce, lambda e, wj=wj, o_out=o_out, o_in=o_in, n=n, j=j: e.tensor_scalar(
                                out=acc[:, o_out:o_out + n], in0=xp[:, o_in + j:o_in + j + n], scalar1=wj, scalar2=None,
                                op0=ALU.mult), outs=[acc_t], ins=[xp_t, sm_t])
                        else:
                            kb.op(ce, lambda e, wj=wj, o_out=o_out, o_in=o_in, n=n, j=j: e.scalar_tensor_tensor(
                                out=acc[:, o_out:o_out + n], in0=xp[:, o_in + j:o_in + j + n], scalar=wj,
                                in1=acc[:, o_out:o_out + n], op0=ALU.mult, op1=ALU.add), outs=[acc_t], ins=[xp_t, sm_t, acc_t])
                if QS == 2: break
                kb.op(ACT, lambda e: e.activation(out=acc[:], in_=acc[:], func=AF.Silu), outs=[acc_t], ins=[acc_t])
                if QS == 3: break
                if comp < 2:
                    kb.op(ACT, lambda e: e.activation(out=sq[:], in_=acc[:], func=AF.Square), outs=[sq_t], ins=[acc_t])
                    for t0 in range(0, TA, 512):
                        n = min(512, TA - t0)
                        bt, bt_t = kb.bank()
                        kb.op(PE, lambda e, bt=bt, t0=t0, n=n: e.matmul(bt[:, 0:n], ones, sq[:, t0:t0 + n], start=True,
                                                                         stop=True), outs=[bt_t], ins=[sq_t, cst_t])
                        kb.op(ACT, lambda e, bt=bt, n=n: e.activation(out=rn[:, 0:n], in_=bt[:, 0:n], func=AF.Sqrt,
                                                                      bias=eps_c, scale=1.0), outs=[rn_t], ins=[bt_t, col_t])
                        kb.op(DVE, lambda e, n=n: e.reciprocal(out=rn[:, 0:n], in_=rn[:, 0:n]), outs=[rn_t], ins=[rn_t])
                        sc_ = (128.0 ** -0.5) if comp == 0 else 1.0
                        kb.op(DVE, lambda e, t0=t0, n=n, sc_=sc_: e.scalar_tensor_tensor(
                            out=acc[:, t0:t0 + n], in0=acc[:, t0:t0 + n], scalar=sc_, in1=rn[:, 0:n], op0=ALU.mult,
                            op1=ALU.mult), outs=[acc_t], ins=[acc_t, rn_t])
                    if QS == 4: break
                    kb.dma((qTd if comp == 0 else kTd)[h], acc[:], rtok=acc_t)
                    if QS == 5: break
                if comp >= 1:
                    dst = ktokd if comp == 1 else vtokd
                    for gi, t0 in enumerate(range(0, NTA, 4)):
                        ng = min(4, NTA - t0)
                        bt, bt_t = kb.bank()
                        for k in range(ng):
                            kb.op(PE, lambda e, bt=bt, k=k, t0=t0: e.transpose(
                                out=bt[:, k * 128:(k + 1) * 128], in_=acc[:, (t0 + k) * 128:(t0 + k + 1) * 128],
                                identity=ident), outs=[bt_t], ins=[acc_t, cst_t])
                        st, st_t = sts[gi % 2]
                        kb.op(ACT if gi % 2 == 0 else DVE,
                              (lambda e, bt=bt, st=st, ng=ng: e.activation(out=st[:, 0:ng].rearrange("p n t -> p (n t)"),
                                                                           in_=bt[:, 0:ng * 128], func=AF.Copy))
                              if gi % 2 == 0 else
                              (lambda e, bt=bt, st=st, ng=ng: e.tensor_copy(out=st[:, 0:ng].rearrange("p n t -> p (n t)"),
                                                                            in_=bt[:, 0:ng * 128])),
                              outs=[st_t], ins=[bt_t])
                        kb.dma(dst[h, t0:t0 + ng].rearrange("n t d -> t n d"), st[:, 0:ng], rtok=st_t)
        kb.end()

    def deltanet_phase(l, gate_b, gate_g, gt_t):
        kb.begin()
        qf = kb.sb([128, TA]); qf_t = kb.tok()
        kf = kb.sb([128, TA]); kf_t = kb.tok()
        kt = kb.sb([128, NTA, 128]); kt_t = kb.tok()
        vt = kb.sb([128, NTA, 128]); vt_t = kb.tok()
        oacc = kb.sb([128, NTA, 128]); o_ts = [kb.tok() for _ in range(NTA)]
        oT = qf; oT_t = qf_t
        X1 = kf; X1_t = kf_t
        X2 = kt[:].rearrange("p n t -> p (n t)"); X2_t = kt_t
        cbs = [(kb.sb([128, 512], BF16), kb.tok()) for _ in range(2)]
        rnb = kb.sb([128, 512]); rnb_t = kb.tok()
        gcs = kb.sb([128, 2, 3, NTA]); gcs_t = kb.tok()
        Sst = [(kb.sb([128, 128]), kb.tok()) for _ in range(2)]
        NSLOT = 3
        names = ("E", "ET", "qd", "N", "M", "N2", "M2", "S", "TTw")
        alias = dict(Gb="E", Dn="E", Dt="ET", eR="qd", TTu="E", U="N", WT="M", kd="N2", vn="M2", qk="ET")
        slots = []
        for _d in range(2):
            row = []
            for _s in range(NSLOT):
                dct = {nm: (kb.sb([128, 128]), kb.tok()) for nm in names}
                for a_, b_ in alias.items():
                    dct[a_] = dct[b_]
                row.append(dct)
            slots.append(row)
        cols = [[(kb.sb([128, 4]), kb.tok()) for _ in range(NSLOT)] for _ in range(2)]
        order = [list(range(NTL, NTA)) + list(range(0, NTL)),
                 list(range(NTA - 1, NTL - 1, -1)) + list(range(NTL - 1, -1, -1))]
        dmask = [dict(cs=m_le, rr=m_le, am=m_gt, qm=m_le, last=127), dict(cs=m_ge, rr=m_ge, am=m_lt, qm=m_ge, last=0)]
        ee = [0]

        def alt():
            ee[0] += 1
            return DVE if ee[0] % 2 == 0 else POOL

        for h in range(H):
            kb.dma(qf[:], qTd[h], wtok=qf_t)
            kb.dma(kf[:], kTd[h], wtok=kf_t)
            kb.dma(kt[:], ktokd[h].rearrange("n t d -> t n d"), wtok=kt_t)
            kb.dma(vt[:], vtokd[h].rearrange("n t d -> t n d"), wtok=vt_t)
            for ti in range(NTA):
                kb.op(POOL, lambda e, ti=ti: e.memset(oacc[:, ti, :], 0.0), outs=[o_ts[ti]])
            for d in range(2):
                gi = d * H + h
                bt, bt_t = kb.bank()
                kb.op(PE, lambda e, bt=bt, d=d, gi=gi: e.matmul(bt[:, 0:NTA], dmask[d]["cs"], gate_g[:, :, gi],
                                                                 start=True, stop=True), outs=[bt_t], ins=[gt_t, cst_t])
                kb.op(DVE, lambda e, bt=bt, d=d: e.tensor_copy(out=gcs[:, d, 0, :], in_=bt[:, 0:NTA]), outs=[gcs_t], ins=[bt_t])
                kb.op(ACT, lambda e, d=d: e.activation(out=gcs[:, d, 1, :], in_=gcs[:, d, 0, :], func=AF.Exp),
                      outs=[gcs_t], ins=[gcs_t])
                kb.op(DVE, lambda e, d=d, gi=gi: e.tensor_tensor(out=gcs[:, d, 1, :], in0=gcs[:, d, 1, :],
                                                                 in1=gate_b[:, :, gi], op=ALU.mult),
                      outs=[gcs_t], ins=[gcs_t, gt_t])
                kb.op(DVE, lambda e, d=d, gi=gi: e.tensor_scalar(out=gcs[:, d, 2, :], in0=gate_b[:, :, gi], scalar1=-1.0,
                                                                 scalar2=None, op0=ALU.mult), outs=[gcs_t], ins=[gt_t])
                S0, S0_t = Sst[d]
                kb.op(POOL, lambda e, S0=S0: e.memset(S0[:], 0.0), outs=[S0_t])

            def pre(d, step):
                ci = order[d][step]
                sl = slots[d][step % NSLOT]
                cl, cl_t = cols[d][step % NSLOT]
                mk_ = dmask[d]
                gi = d * H + h
                g_col = gate_g[:, ci, gi:gi + 1]
                b_col = gate_b[:, ci, gi:gi + 1]
                gc_col = gcs[:, d, 0, ci:ci + 1]
                bw_col = gcs[:, d, 1, ci:ci + 1]
                nb_col = gcs[:, d, 2, ci:ci + 1]
                kT_c = kf[:, ci * 128:(ci + 1) * 128]
                qT_c = qf[:, ci * 128:(ci + 1) * 128]
                T = lambda nm: sl[nm][0]
                K = lambda nm: sl[nm][1]
                import os as _os
                PS = int(_os.environ.get('PSUB', '100'))
                kb.op(alt(), lambda e: e.tensor_scalar(out=T("Gb")[:], in0=ones, scalar1=g_col, scalar2=None, op0=ALU.mult),
                      outs=[K("Gb")], ins=[cst_t, gt_t])
                bR, bR_t = kb.bank()
                kb.op(PE, lambda e: e.matmul(bR[:, 0:128], T("Gb")[:], mk_["rr"], start=True, stop=True),
                      outs=[bR_t], ins=[K("Gb"), cst_t])
                if PS == 1: return
                kb.op(DVE, lambda e: e.tensor_scalar(out=T("Dn")[:], in0=bR[:, 0:128], scalar1=gc_col, scalar2=zero_c,
                                                     op0=ALU.subtract, op1=ALU.max), outs=[K("Dn")], ins=[bR_t, gcs_t])
                kb.op(ACT, lambda e: e.activation(out=T("E")[:], in_=T("Dn")[:], func=AF.Exp, scale=-1.0),
                      outs=[K("E")], ins=[K("Dn")])
                kb.op(DVE, lambda e: e.tensor_scalar(out=T("Dt")[:], in0=bR[:, 0:128], scalar1=gc_col, scalar2=zero_c,
                                                     op0=ALU.subtract, op1=ALU.min), outs=[K("Dt")], ins=[bR_t, gcs_t])
                kb.op(ACT, lambda e: e.activation(out=T("ET")[:], in_=T("Dt")[:], func=AF.Exp), outs=[K("ET")], ins=[K("Dt")])
                if PS == 2: return
                lc = mk_["last"]
                kb.op(ACT, lambda e: e.activation(out=cl[:, 0:1], in_=bR[:, lc:lc + 1], func=AF.Exp), outs=[cl_t], ins=[bR_t])
                kb.op(DVE, lambda e: e.tensor_tensor(out=cl[:, 1:2], in0=bR[:, lc:lc + 1], in1=gc_col, op=ALU.subtract),
                      outs=[cl_t], ins=[bR_t, gcs_t])
                kb.op(ACT, lambda e: e.activation(out=cl[:, 1:2], in_=cl[:, 1:2], func=AF.Exp), outs=[cl_t], ins=[cl_t])
                kb.op(ACT, lambda e: e.activation(out=T("eR")[:], in_=bR[:, 0:128], func=AF.Exp), outs=[K("eR")], ins=[bR_t])
                kb.op(alt(), lambda e: e.tensor_tensor(out=T("qd")[:], in0=qT_c, in1=T("eR")[:], op=ALU.mult),
                      outs=[K("qd")], ins=[qf_t, K("eR")])
                if PS == 3: return
                bK, bK_t = kb.bank()
                kb.op(PE, lambda e: e.matmul(bK[:, 0:128], kT_c, kT_c, start=True, stop=True), outs=[bK_t], ins=[kf_t])
                kb.op(POOL, lambda e: e.tensor_tensor(out=T("E")[:], in0=T("E")[:], in1=mk_["am"], op=ALU.mult),
                      outs=[K("E")], ins=[K("E"), cst_t])
                kb.op(DVE, lambda e: e.scalar_tensor_tensor(out=T("N")[:], in0=bK[:, 0:128], scalar=nb_col, in1=T("E")[:],
                                                            op0=ALU.mult, op1=ALU.mult), outs=[K("N")], ins=[bK_t, K("E"), gcs_t])
                if PS == 4: return
                bM, bM_t = kb.bank()
                kb.op(PE, lambda e: e.transpose(out=bM[:, 0:128], in_=T("N")[:], identity=ident), outs=[bM_t], ins=[K("N"), cst_t])
                kb.op(ACT, lambda e: e.activation(out=T("M")[:], in_=bM[:, 0:128], func=AF.Copy), outs=[K("M")], ins=[bM_t])
                kb.op(DVE, lambda e: e.tensor_tensor(out=T("S")[:], in0=bM[:, 0:128], in1=ident, op=ALU.add),
                      outs=[K("S")], ins=[bM_t, cst_t])
                if PS == 5: return
                pN, pM = "N", "M"
                for lev in range(1, 7):
                    nN = "N2" if pN == "N" else "N"
                    nM = "M2" if pM == "M" else "M"
                    bn, bn_t = kb.bank()
                    kb.op(PE, lambda e, bn=bn, pN=pN, pM=pM: e.matmul(bn[:, 0:128], T(pM)[:], T(pN)[:], start=True, stop=True),
                          outs=[bn_t], ins=[K(pM), K(pN)])
                    if lev < 6:
                        bm, bm_t = kb.bank()
                        kb.op(PE, lambda e, bm=bm, pN=pN, pM=pM: e.matmul(bm[:, 0:128], T(pN)[:], T(pM)[:], start=True, stop=True),
                              outs=[bm_t], ins=[K(pM), K(pN)])
                    kb.op(ACT, lambda e, bn=bn, nN=nN: e.activation(out=T(nN)[:], in_=bn[:, 0:128], func=AF.Copy),
                          outs=[K(nN)], ins=[bn_t])
                    if lev < 6:
                        kb.op(DVE, lambda e, bm=bm, nM=nM: e.tensor_copy(out=T(nM)[:], in_=bm[:, 0:128]), outs=[K(nM)], ins=[bm_t])
                    bs, bs_t = kb.bank()
                    kb.op(PE, lambda e, bs=bs, nN=nN: e.matmul(bs[:, 0:128], T(nN)[:], T("S")[:], start=True, stop=True),
                          outs=[bs_t], ins=[K(nN), K("S")])
                    kb.op(DVE, lambda e, bs=bs: e.tensor_tensor(out=T("S")[:], in0=bs[:, 0:128], in1=T("S")[:], op=ALU.add),
                          outs=[K("S")], ins=[bs_t, K("S")])
                    pN, pM = nN, nM
                if PS == 6: return
                kb.op(ACT, lambda e: e.activation(out=T("TTu")[:], in_=T("S")[:], func=AF.Copy, scale=b_col),
                      outs=[K("TTu")], ins=[K("S"), gt_t])
                kb.op(POOL, lambda e: e.tensor_scalar(out=T("TTw")[:], in0=T("S")[:], scalar1=bw_col, scalar2=None, op0=ALU.mult),
                      outs=[K("TTw")], ins=[K("S"), gcs_t])
                bU, bU_t = kb.bank()
                kb.op(PE, lambda e: e.matmul(bU[:, 0:128], T("TTu")[:], vt[:, ci, :], start=True, stop=True),
                      outs=[bU_t], ins=[K("TTu"), vt_t])
                kb.op(ACT, lambda e: e.activation(out=T("U")[:], in_=bU[:, 0:128], func=AF.Copy), outs=[K("U")], ins=[bU_t])
                bW, bW_t = kb.bank()
                kb.op(PE, lambda e: e.matmul(bW[:, 0:128], kt[:, ci, :], T("TTw")[:], start=True, stop=True),
                      outs=[bW_t], ins=[K("TTw"), kt_t])
                kb.op(DVE, lambda e: e.tensor_copy(out=T("WT")[:], in_=bW[:, 0:128]), outs=[K("WT")], ins=[bW_t])
                if PS == 7: return
                bQ, bQ_t = kb.bank()
                kb.op(PE, lambda e: e.matmul(bQ[:, 0:128], kT_c, qT_c, start=True, stop=True), outs=[bQ_t], ins=[kf_t, qf_t])
                kb.op(POOL, lambda e: e.tensor_tensor(out=T("ET")[:], in0=T("ET")[:], in1=mk_["qm"], op=ALU.mult),
                      outs=[K("ET")], ins=[K("ET"), cst_t])
                kb.op(DVE, lambda e: e.tensor_tensor(out=T("qk")[:], in0=bQ[:, 0:128], in1=T("ET")[:], op=ALU.mult),
                      outs=[K("qk")], ins=[bQ_t, K("ET")])
                kb.op(POOL, lambda e: e.tensor_scalar(out=T("kd")[:], in0=kt[:, ci, :], scalar1=cl[:, 1:2], scalar2=None,
                                                      op0=ALU.mult), outs=[K("kd")], ins=[kt_t, cl_t])

            def seq(d, step):
                ci = order[d][step]
                sl = slots[d][step % NSLOT]
                cl, cl_t = cols[d][step % NSLOT]
                T = lambda nm: sl[nm][0]
                K = lambda nm: sl[nm][1]
                S_, S_t = Sst[d]
                b1, b1_t = kb.bank()
                kb.op(PE, lambda e: e.matmul(b1[:, 0:128], T("WT")[:], S_[:], start=True, stop=True),
                      outs=[b1_t], ins=[K("WT"), S_t])
                b2, b2_t = kb.bank()
                kb.op(PE, lambda e: e.matmul(b2[:, 0:128], T("qd")[:], S_[:], start=True, stop=False),
                      outs=[b2_t], ins=[K("qd"), S_t])
                kb.op(DVE, lambda e: e.tensor_tensor(out=T("vn")[:], in0=T("U")[:], in1=b1[:, 0:128], op=ALU.subtract),
                      outs=[K("vn")], ins=[K("U"), b1_t])
                kb.op(PE, lambda e: e.matmul(b2[:, 0:128], T("qk")[:], T("vn")[:], start=False, stop=True),
                      outs=[b2_t], ins=[K("qk"), K("vn")])
                b3, b3_t = kb.bank()
                kb.op(PE, lambda e: e.matmul(b3[:, 0:128], T("kd")[:], T("vn")[:], start=True, stop=True),
                      outs=[b3_t], ins=[K("kd"), K("vn")])
                kb.op(DVE, lambda e: e.scalar_tensor_tensor(out=S_[:], in0=S_[:], scalar=cl[:, 0:1], in1=b3[:, 0:128],
                                                            op0=ALU.mult, op1=ALU.add), outs=[S_t], ins=[S_t, b3_t, cl_t])
                kb.op(POOL if False else DVE, lambda e: e.tensor_tensor(out=oacc[:, ci, :], in0=b2[:, 0:128], in1=oacc[:, ci, :],
                                                                        op=ALU.add), outs=[o_ts[ci]], ins=[b2_t, o_ts[ci]])

            import os as _os
            DS = int(_os.environ.get("DSUB", "100"))
            if DS == 1: continue
            pre(0, 0)
            if DS == 2: continue
            pre(1, 0)
            if DS == 3: continue
            for step in range(NTA):
                if step + 1 < NTA:
                    pre(0, step + 1)
                seq(0, step)
                if step + 1 < NTA:
                    pre(1, step + 1)
                seq(1, step)
            if DS == 4: continue
            for t0 in range(0, NTA, 4):
                ng = min(4, NTA - t0)
                bt, bt_t = kb.bank()
                for k in range(ng):
                    kb.op(PE, lambda e, bt=bt, k=k, t0=t0: e.transpose(out=bt[:, k * 128:(k + 1) * 128], in_=oacc[:, t0 + k, :],
                                                                       identity=ident), outs=[bt_t], ins=[o_ts[t0 + k], cst_t])
                kb.op(ACT, lambda e, bt=bt, t0=t0, ng=ng: e.activation(out=oT[:, t0 * 128:(t0 + ng) * 128], in_=bt[:, 0:ng * 128],
                                                                       func=AF.Copy), outs=[oT_t], ins=[bt_t])
            kb.op(ACT, lambda e: e.activation(out=X1[:], in_=oT[:], func=AF.Square), outs=[X1_t], ins=[oT_t])
            r0 = c["OFF_Z"] + h * 128
            kb.dma(X2, pT[r0:r0 + 128, :], wtok=X2_t)
            kb.op(ACT, lambda e: e.activation(out=X2, in_=X2, func=AF.Silu), outs=[X2_t], ins=[X2_t])
            for gi2, t0 in enumerate(range(0, NTA, 4)):
                ng = min(4, NTA - t0)
                n = ng * 128
                a0 = t0 * 128
                bt, bt_t = kb.bank()
                kb.op(PE, lambda e, bt=bt, a0=a0, n=n: e.matmul(bt[:, 0:n], ones, X1[:, a0:a0 + n], start=True, stop=True),
                      outs=[bt_t], ins=[X1_t, cst_t])
                kb.op(ACT, lambda e, bt=bt, n=n: e.activation(out=rnb[:, 0:n], in_=bt[:, 0:n], func=AF.Sqrt, bias=eps_c,
                                                              scale=1.0 / 128.0), outs=[rnb_t], ins=[bt_t, col_t])
                kb.op(DVE, lambda e, n=n: e.reciprocal(out=rnb[:, 0:n], in_=rnb[:, 0:n]), outs=[rnb_t], ins=[rnb_t])
                kb.op(DVE, lambda e, a0=a0, n=n: e.scalar_tensor_tensor(out=rnb[:, 0:n], in0=oT[:, a0:a0 + n],
                                                                        scalar=dnn_s[:, l:l + 1], in1=rnb[:, 0:n], op0=ALU.mult,
                                                                        op1=ALU.mult), outs=[rnb_t], ins=[oT_t, rnb_t, sm_t])
                cb, cb_t = cbs[gi2 % 2]
                kb.op(POOL, lambda e, cb=cb, a0=a0, n=n: e.tensor_tensor(out=cb[:, 0:n], in0=rnb[:, 0:n], in1=X2[:, a0:a0 + n],
                                                                         op=ALU.mult), outs=[cb_t], ins=[rnb_t, X2_t])
                kb.dma(catT[t0:t0 + ng, :, h, :].rearrange("n p t -> p n t"), cb[:, 0:n].rearrange("p (n t) -> p n t", t=128),
                       rtok=cb_t)
        kb.end()

    def conformer_phase(l):
        kb.begin()
        als = [(kb.sb([128, ROWS, GW]), kb.sb([128, TC]), kb.tok()) for _ in range(1)]
        gls = [(kb.sb([128, ROWS, GW]), kb.sb([128, TC]), kb.tok()) for _ in range(1)]
        uls = [(kb.sb([128, ROWS, GW]), kb.sb([128, TC]), kb.tok()) for _ in range(1)]
        yls = [(kb.sb([128, ROWS, GW]), kb.sb([128, TC]), kb.tok()) for _ in range(1)]
        st1 = kb.sb([128, TA]); st2 = kb.sb([128, TA]); st_t = kb.tok()
        sqb = kb.sb([128, 512]); sqb_t = kb.tok()
        kb.op(DVE, lambda e: e.memset(st1[:], 0.0), outs=[st_t])
        kb.op(DVE, lambda e: e.memset(st2[:], 0.0), outs=[st_t])
        RB = 512 // GW
        for ct in range(CT):
            al, ac, a_t = als[0]
            gl, gc_, g_t = gls[0]
            ul, uc, u_t = uls[0]
            yl, yc, y_t = yls[0]
            ce = DVE
            ra = c["OFF_A"] + ct * 128
            rg = c["OFF_G"] + ct * 128
            kb.dma(al[:], pT[ra:ra + 128, 0:TL].rearrange("p (r c) -> p r c", c=GW), wtok=a_t)
            kb.dma(ac[:], pT[ra:ra + 128, TL:TA], wtok=a_t)
            kb.dma(gl[:], pT[rg:rg + 128, 0:TL].rearrange("p (r c) -> p r c", c=GW), wtok=g_t)
            kb.dma(gc_[:], pT[rg:rg + 128, TL:TA], wtok=g_t)
            kb.op(ACT, lambda e, gl=gl: e.activation(out=gl[:], in_=gl[:], func=AF.Sigmoid), outs=[g_t], ins=[g_t])
            kb.op(ACT, lambda e, gc_=gc_: e.activation(out=gc_[:], in_=gc_[:], func=AF.Sigmoid), outs=[g_t], ins=[g_t])
            kb.op(ce, lambda e, ul=ul, al=al, gl=gl: e.tensor_tensor(out=ul[:], in0=al[:], in1=gl[:], op=ALU.mult),
                  outs=[u_t], ins=[a_t, g_t])
            kb.op(ce, lambda e, uc=uc, ac=ac, gc_=gc_: e.tensor_tensor(out=uc[:], in0=ac[:], in1=gc_[:], op=ALU.mult),
                  outs=[u_t], ins=[a_t, g_t])
            w0 = (l * CT + ct) * 31
            wc = lambda j: cdw_s[:, w0 + j:w0 + j + 1]
            bcol = cvec_s[:, (l * 3 + 0) * CT + ct:(l * 3 + 0) * CT + ct + 1]
            kb.op(ce, lambda e, yl=yl, ul=ul: e.tensor_scalar(out=yl[:], in0=ul[:], scalar1=wc(15), scalar2=bcol, op0=ALU.mult,
                                                              op1=ALU.add), outs=[y_t], ins=[u_t, sm_t])
            kb.op(ce, lambda e, yc=yc, uc=uc: e.tensor_scalar(out=yc[:], in0=uc[:], scalar1=wc(15), scalar2=bcol, op0=ALU.mult,
                                                              op1=ALU.add), outs=[y_t], ins=[u_t, sm_t])
            for j in range(31):
                if j == 15:
                    continue
                s = j - 15
                if ct < CT // 2:
                    c0, c1 = max(0, -s), min(GW, GW - s)
                    if c1 > c0:
                        kb.op(ce, lambda e, yl=yl, ul=ul, c0=c0, c1=c1, s=s, j=j: e.scalar_tensor_tensor(
                            out=yl[:, :, c0:c1], in0=ul[:, :, c0 + s:c1 + s], scalar=wc(j), in1=yl[:, :, c0:c1], op0=ALU.mult,
                            op1=ALU.add), outs=[y_t], ins=[u_t, y_t, sm_t])
                else:
                    r0_, r1_ = max(0, -s), min(ROWS, ROWS - s)
                    if r1_ > r0_:
                        kb.op(ce, lambda e, yl=yl, ul=ul, r0_=r0_, r1_=r1_, s=s, j=j: e.scalar_tensor_tensor(
                            out=yl[:, r0_:r1_, :], in0=ul[:, r0_ + s:r1_ + s, :], scalar=wc(j), in1=yl[:, r0_:r1_, :],
                            op0=ALU.mult, op1=ALU.add), outs=[y_t], ins=[u_t, y_t, sm_t])
                t0_, t1_ = max(0, -s), min(TC, TC - s)
                if t1_ > t0_:
                    kb.op(ce, lambda e, yc=yc, uc=uc, t0_=t0_, t1_=t1_, s=s, j=j: e.scalar_tensor_tensor(
                        out=yc[:, t0_:t1_], in0=uc[:, t0_ + s:t1_ + s], scalar=wc(j), in1=yc[:, t0_:t1_], op0=ALU.mult,
                        op1=ALU.add), outs=[y_t], ins=[u_t, y_t, sm_t])
            blocks = [(yl[:, r:r + RB, :], r * GW, RB * GW, True) for r in range(0, ROWS, RB)]
            blocks += [(yc[:, t:min(TC, t + 512)], TL + t, min(512, TC - t), False) for t in range(0, TC, 512)]
            for (ap_, o0, n, is3) in blocks:
                sqv = sqb[:, 0:n].rearrange("p (r c) -> p r c", c=GW) if is3 else sqb[:, 0:n]
                kb.op(ACT, lambda e, ap_=ap_, sqv=sqv: e.activation(out=sqv, in_=ap_, func=AF.Square), outs=[sqb_t], ins=[y_t])
                b1, b1_t = kb.bank()
                kb.op(PE, lambda e, b1=b1, ap_=ap_, n=n: e.matmul(b1[:, 0:n], ones, ap_, start=True, stop=True),
                      outs=[b1_t], ins=[y_t, cst_t])
                b2, b2_t = kb.bank()
                kb.op(PE, lambda e, b2=b2, n=n: e.matmul(b2[:, 0:n], ones, sqb[:, 0:n], start=True, stop=True),
                      outs=[b2_t], ins=[sqb_t, cst_t])
                kb.op(DVE, lambda e, b1=b1, o0=o0, n=n: e.tensor_tensor(out=st1[:, o0:o0 + n], in0=b1[:, 0:n], in1=st1[:, o0:o0 + n],
                                                                        op=ALU.add), outs=[st_t], ins=[b1_t, st_t])
                kb.op(DVE, lambda e, b2=b2, o0=o0, n=n: e.tensor_tensor(out=st2[:, o0:o0 + n], in0=b2[:, 0:n], in1=st2[:, o0:o0 + n],
                                                                        op=ALU.add), outs=[st_t], ins=[b2_t, st_t])
            kb.dma(yTd[ct * 128:(ct + 1) * 128, 0:TL].rearrange("p (r c) -> p r c", c=GW), yl[:], rtok=y_t)
            kb.dma(yTd[ct * 128:(ct + 1) * 128, TL:TA], yc[:], rtok=y_t)
        mu = kb.sb([128, TA]); mu_t = kb.tok()
        kb.op(DVE, lambda e: e.tensor_scalar(out=mu[:], in0=st1[:], scalar1=1.0 / CW, scalar2=None, op0=ALU.mult),
              outs=[mu_t], ins=[st_t])
        kb.op(DVE, lambda e: e.tensor_scalar(out=st2[:], in0=st2[:], scalar1=1.0 / CW, scalar2=None, op0=ALU.mult),
              outs=[st_t], ins=[st_t])
        kb.op(DVE, lambda e: e.tensor_tensor(out=st1[:], in0=mu[:], in1=mu[:], op=ALU.mult), outs=[st_t], ins=[mu_t, st_t])
        kb.op(DVE, lambda e: e.tensor_tensor(out=st2[:], in0=st2[:], in1=st1[:], op=ALU.subtract), outs=[st_t], ins=[st_t])
        kb.op(DVE, lambda e: e.tensor_scalar(out=st2[:], in0=st2[:], scalar1=0.0, scalar2=None, op0=ALU.max),
              outs=[st_t], ins=[st_t])
        kb.op(ACT, lambda e: e.activation(out=st1[:], in_=st2[:], func=AF.Sqrt, bias=eps_c, scale=1.0), outs=[st_t],
              ins=[st_t, col_t])
        kb.op(DVE, lambda e: e.reciprocal(out=st1[:], in_=st1[:]), outs=[st_t], ins=[st_t])
        kb.op(DVE, lambda e: e.scalar_tensor_tensor(out=st2[:], in0=mu[:], scalar=-1.0, in1=st1[:], op0=ALU.mult, op1=ALU.mult),
              outs=[st_t], ins=[mu_t, st_t])
        kb.dma(lnst[0], st1[:], rtok=st_t)
        kb.dma(lnst[1], st2[:], rtok=st_t)
        kb.end()
        kb.begin()
        st1 = kb.sb([128, TA]); st2 = kb.sb([128, TA]); st_t = kb.tok()
        kb.dma(st1[:], lnst[0], wtok=st_t)
        kb.dma(st2[:], lnst[1], wtok=st_t)
        ys = [(kb.sb([128, TA]), kb.tok()) for _ in range(2)]
        czs = [(kb.sb([128, TA], BF16), kb.tok()) for _ in range(2)]
        for ct in range(CT):
            y, y_t = ys[ct % 2]
            cz, cz_t = czs[ct % 2]
            ce = DVE
            kb.dma(y[:], yTd[ct * 128:(ct + 1) * 128, :], wtok=y_t)
            kb.op(ce, lambda e, y=y: e.tensor_tensor(out=y[:], in0=y[:], in1=st1[:], op=ALU.mult), outs=[y_t], ins=[y_t, st_t])
            kb.op(ce, lambda e, y=y: e.tensor_tensor(out=y[:], in0=y[:], in1=st2[:], op=ALU.add), outs=[y_t], ins=[y_t, st_t])
            gcol = cvec_s[:, (l * 3 + 1) * CT + ct:(l * 3 + 1) * CT + ct + 1]
            bcol2 = cvec_s[:, (l * 3 + 2) * CT + ct:(l * 3 + 2) * CT + ct + 1]
            kb.op(ACT, lambda e, y=y, cz=cz, gcol=gcol, bcol2=bcol2: e.activation(out=cz[:], in_=y[:], func=AF.Silu, bias=bcol2,
                                                                                 scale=gcol), outs=[cz_t], ins=[y_t, sm_t])
            kb.dma(catT[:, :, H + ct, :].rearrange("n p t -> p n t"), cz[:].rearrange("p (n t) -> p n t", t=128), rtok=cz_t)
        kb.end()

    def topk_phase():
        kb.begin()
        cmp_ = kb.sb([16, TL]); cmp_t = kb.tok()
        sc = kb.sb([16, 8]); sc_t = kb.tok()
        lo, hi, mid, cnt, ge, d1 = (sc[:, i:i + 1] for i in range(6))
        for (a0, n, k) in ((0, TL, c["CAPL"]), (TL, TC, c["CAPC"])):
            kb.op(DVE, lambda e: e.memset(lo, 0.0), outs=[sc_t])
            kb.op(DVE, lambda e: e.memset(hi, 1.0), outs=[sc_t], ins=[sc_t])
            for it in range(32):
                kb.op(DVE, lambda e: e.tensor_tensor(out=mid, in0=lo, in1=hi, op=ALU.add), outs=[sc_t], ins=[sc_t])
                kb.op(DVE, lambda e: e.tensor_scalar(out=mid, in0=mid, scalar1=0.5, scalar2=None, op0=ALU.mult), outs=[sc_t], ins=[sc_t])
                kb.op(DVE, lambda e, a0=a0, n=n: e.tensor_scalar(out=cmp_[:, 0:n], in0=affT[:, a0:a0 + n], scalar1=mid, scalar2=None,
                                                                 op0=ALU.is_ge), outs=[cmp_t], ins=[aff_t, sc_t])
                kb.op(DVE, lambda e, n=n: e.reduce_sum(out=cnt, in_=cmp_[:, 0:n], axis=AX.X), outs=[sc_t], ins=[cmp_t, sc_t])
                kb.op(DVE, lambda e, k=k: e.tensor_scalar(out=ge, in0=cnt, scalar1=float(k) - 0.5, scalar2=None, op0=ALU.is_gt),
                      outs=[sc_t], ins=[sc_t])
                kb.op(DVE, lambda e: e.tensor_tensor(out=d1, in0=mid, in1=lo, op=ALU.subtract), outs=[sc_t], ins=[sc_t])
                kb.op(DVE, lambda e: e.scalar_tensor_tensor(out=lo, in0=d1, scalar=ge, in1=lo, op0=ALU.mult, op1=ALU.add),
                      outs=[sc_t], ins=[sc_t])
                kb.op(DVE, lambda e: e.tensor_tensor(out=d1, in0=hi, in1=mid, op=ALU.subtract), outs=[sc_t], ins=[sc_t])
                kb.op(DVE, lambda e: e.scalar_tensor_tensor(out=hi, in0=d1, scalar=ge, in1=mid, op0=ALU.mult, op1=ALU.add),
                      outs=[sc_t], ins=[sc_t])
            kb.op(DVE, lambda e, a0=a0, n=n: e.scalar_tensor_tensor(out=gwT[:, a0:a0 + n], in0=affT[:, a0:a0 + n], scalar=lo,
                                                                    in1=affT[:, a0:a0 + n], op0=ALU.is_ge, op1=ALU.mult),
                  outs=[gw_t], ins=[aff_t, sc_t])
        kb.end()

    gate_b = kb.sb([128, NTA, 2 * H], glob=True)
    gate_g = kb.sb([128, NTA, 2 * H], glob=True)
    gt_t = kb.tok(True)
    hT_tiles = [hT[i] for i in range(NTA)]

    import os as _os
    _stop = int(_os.environ.get("KSTOP", "100000"))
    steps = []
    for l in range(L):
        cct = cc_t[l + 1]
        steps.append(lambda l=l: norm_phase(lambda kc, ic, l=l: mcol(l, 1, kc, ic), lambda kc, ic, l=l: mcol(l, 0, kc, ic),
                                            dst_tiles=hT_tiles))
        steps.append(lambda l=l, cct=cct: linear_phase(hT, KC, wview(l, 0, NBIN, KC), NBIN, cct, epi_store(pT)))
        steps.append(lambda l=l: gates_phase(l, gate_b, gate_g, gt_t))
        steps.append(lambda l=l: qkv_phase(l))
        steps.append(lambda l=l: deltanet_phase(l, gate_b, gate_g, gt_t))
        steps.append(lambda l=l: conformer_phase(l))
        steps.append(lambda l=l, cct=cct: linear_phase(catT, KCM, wview(l, 1, KC, KCM), KC, cct, epi_resid(l, 2)))
        steps.append(lambda l=l: norm_phase(lambda kc, ic, l=l: mcol(l, 4, kc, ic), lambda kc, ic, l=l: mcol(l, 3, kc, ic),
                                            dst_tiles=hT_tiles, router_l=l))
        steps.append(lambda l=l: topk_phase())

        def _moe_up(l=l, cct=cct):
            em, pre = epi_moe()
            linear_phase(hT, KC, wview(l, 2, 2 * KCE, KC), 2 * KCE, cct, em, pair=True, pre_super=pre)
        steps.append(_moe_up)
        steps.append(lambda l=l, cct=cct: linear_phase(actT, KCE, wview(l, 3, KC, KCE), KC, cct, epi_resid(l, 5)))
    steps.append(lambda: norm_phase(lambda kc, ic: fn_s[:, kc:kc + 1], lambda kc, ic: None, dst_f32=outT, ntiles=NTL))
    for i, st in enumerate(steps):
        if i >= _stop:
            break
        st()
    kb.barrier()
    return nc, c


def blocks_layout(W, nb, kck):
    K, N = W.shape
    Wp = np.zeros((kck * 128, nb * 128), np.float32)
    Wp[:K, :N] = W
    return np.ascontiguousarray(Wp.reshape(kck, 128, nb, 128).transpose(2, 1, 0, 3).reshape(nb * 128, kck * 128))


def build_ada(cfg):
    c = derive(cfg)
    kb = KB(c)
    nc = kb.nc
    PE, DVE, ACT = kb.PE, kb.DVE, kb.ACT
    D, KC, L, NSH = c["D"], c["KC"], c["L"], c["NSH"]
    condT = nc.dram_tensor("condT", [D, 5], F32, kind="ExternalInput").ap()
    wada = nc.dram_tensor("wada", [L * KC * 128, NSH], F32, kind="ExternalInput").ap()
    bada = nc.dram_tensor("bada", [L, NSH], F32, kind="ExternalInput").ap()
    mods = nc.dram_tensor("mods", [L * 5, NSH], F32, kind="ExternalOutput").ap()
    kb.begin()
    onesr = kb.sb([1, 8]); on_t = kb.tok()
    kb.op(DVE, lambda e: e.memset(onesr[:], 1.0), outs=[on_t])
    scT = kb.sb([128, KC, 5]); sc_t = kb.tok()
    kb.dma(scT[:], condT.rearrange("(k p) i -> p k i", p=128), wtok=sc_t)
    kb.op(ACT, lambda e: e.activation(out=scT[:], in_=scT[:], func=AF.Silu), outs=[sc_t], ins=[sc_t])
    NBK = min(512, NSH)
    NB6 = NSH // NBK
    wts = [(kb.sb([128, NSH]), kb.tok()) for _ in range(2)]
    brow = kb.sb([1, NSH]); br_t = kb.tok()
    mrow = kb.sb([5, NSH]); mr_t = kb.tok()
    for l in range(L):
        kb.dma(brow[:], bada[l:l + 1, :], wtok=br_t)
        bks = [kb.bank() for _ in range(NB6)]
        for kc in range(KC):
            wt, wt_t = wts[(l * KC + kc) % 2]
            r0 = (l * KC + kc) * 128
            kb.dma(wt[:], wada[r0:r0 + 128, :], wtok=wt_t)
            for j in range(NB6):
                bt, bt_t = bks[j]
                kb.op(PE, lambda e, bt=bt, wt=wt, kc=kc, j=j: e.matmul(
                    bt[0:5, 0:NBK], scT[:, kc, :], wt[:, j * NBK:(j + 1) * NBK], start=(kc == 0), stop=False),
                    outs=[bt_t], ins=[sc_t, wt_t])
        for j in range(NB6):
            bt, bt_t = bks[j]
            kb.op(PE, lambda e, bt=bt, j=j: e.matmul(
                bt[0:5, 0:NBK], onesr[0:1, 0:5], brow[0:1, j * NBK:(j + 1) * NBK], start=False, stop=True),
                outs=[bt_t], ins=[on_t, br_t])
            kb.op(DVE, lambda e, bt=bt, j=j: e.tensor_copy(out=mrow[:, j * NBK:(j + 1) * NBK], in_=bt[0:5, 0:NBK]),
                  outs=[mr_t], ins=[bt_t])
        kb.dma(mods[l * 5:(l + 1) * 5, :], mrow[:], rtok=mr_t)
    kb.end()
    kb.barrier()
    return nc, c


def prep_shared(cfg, inp):
    c = derive(cfg)
    D, KC, TL, TC, TA, H, DNW, CW, CT, L, E_, FF, FT, EF, KCE, NSH, NBIN, KCM = (
        c[k] for k in ("D", "KC", "TL", "TC", "TA", "H", "DNW", "CW", "CT", "L", "E", "FF", "FT", "EF", "KCE", "NSH",
                       "NBIN", "KCM"))
    f = lambda a: np.asarray(a, np.float32)
    shared = {}
    shared["convw"] = np.ascontiguousarray(
        f(inp["conv_qkv"]).reshape(L, 5, 3 * H, 128).transpose(3, 0, 2, 1).reshape(128, L * 3 * H * 5))
    shared["alog_b"] = np.ascontiguousarray(np.broadcast_to(f(inp["a_log"]).reshape(1, L * 2 * H), (128, L * 2 * H)))
    shared["dtb_b"] = np.ascontiguousarray(np.broadcast_to(f(inp["dt_bias"]).reshape(1, L * 2 * H), (128, L * 2 * H)))
    shared["dnn"] = np.ascontiguousarray(f(inp["dn_norm"]).T)
    shared["cdw"] = np.ascontiguousarray(
        f(inp["conf_dw"]).reshape(L, 31, CT, 128).transpose(3, 0, 2, 1).reshape(128, L * CT * 31))
    cv = np.stack([f(inp["conf_dw_b"]), f(inp["conf_ln_g"]), f(inp["conf_ln_b"])], 1)
    shared["cvec"] = np.ascontiguousarray(cv.reshape(L, 3, CT, 128).transpose(3, 0, 1, 2).reshape(128, L * 3 * CT))
    shared["wrt"] = np.ascontiguousarray(
        f(inp["w_router"]).reshape(L, KC, 128, 16).transpose(2, 0, 1, 3).reshape(128, L * KC * 16))
    shared["fnorm"] = np.ascontiguousarray(f(inp["final_norm"]).reshape(KC, 128).T)
    p = np.arange(128)[:, None]
    q = np.arange(128)[None, :]
    shared["consts"] = np.concatenate([(p == q), np.ones((128, 128)), (p <= q), (p >= q), (p < q), (p > q)],
                                      1).astype(np.float32)
    sm = np.zeros((16, 16, 128), np.float32)
    for e in range(16):
        sm[e, e, :] = 1.0
    shared["selmat"] = sm.reshape(16, 16 * 128)
    perm = np.concatenate([np.arange(0, 4 * DNW), np.arange(c["INW"] - 2 * CW, c["INW"]),
                           np.arange(4 * DNW, 4 * DNW + 4 * H)])
    wall = np.zeros((L, c["WFLAT"]), np.float32)
    for l in range(L):
        parts = [blocks_layout(f(inp["w_in"][l])[:, perm], NBIN, KC),
                 blocks_layout(f(inp["w_out"][l]), KC, KCM)]
        wg = f(inp["w_gate_e"][l]); wu = f(inp["w_up_e"][l])
        W1 = np.stack([wg.reshape(E_, D, FT, 128), wu.reshape(E_, D, FT, 128)], 3)
        W1 = W1.transpose(1, 0, 2, 3, 4).reshape(D, E_ * FT * 2 * 128)
        parts.append(blocks_layout(W1, 2 * KCE, KC))
        parts.append(blocks_layout(f(inp["w_down_e"][l]).reshape(EF, D), KC, KCE))
        for i, pa in enumerate(parts):
            wall[l, c["WOFF"][i]:c["WOFF"][i] + pa.size] = pa.ravel()
        del parts, W1
    shared["wall"] = wall.reshape(L * (c["WFLAT"] // 2048), 2048)
    return shared


_CACHE = {}


def run(cfg, inp):
    key = tuple(sorted(cfg.items()))
    if key not in _CACHE:
        _CACHE[key] = (build_ada(cfg), build(cfg))
    (nca, c), (ncm, _) = _CACHE[key]
    f = lambda a: np.asarray(a, np.float32)
    D, KC, L, NSH, B = c["D"], c["KC"], c["L"], c["NSH"], c["B"]
    condT = np.ascontiguousarray(np.concatenate([f(inp["c"]), f(inp["c_ctx"])[None]], 0).T)
    wada = f(inp["w_ada"]).reshape(L, KC * 128, NCORES, NSH)
    bada = f(inp["b_ada"]).reshape(L, NCORES, NSH)
    mapsA = [dict(condT=condT, wada=np.ascontiguousarray(wada[:, :, r, :]).reshape(L * KC * 128, NSH),
                  bada=np.ascontiguousarray(bada[:, r, :])) for r in range(NCORES)]
    resA = run_bass_kernel_spmd(nca, mapsA, core_ids=list(range(NCORES)))
    mods = np.stack([resA.results[r]["mods"].reshape(L, 5, NSH) for r in range(NCORES)], 2).reshape(L, 5, 6, KC, 128)
    del mapsA, wada
    shared = prep_shared(cfg, inp)
    mapsB = []
    for b in range(B):
        m = dict(shared)
        m["xT"] = np.ascontiguousarray(np.concatenate([f(inp["x"][b]).T, f(inp["ctx"][b]).T], 1))
        msel = np.stack([mods[:, b], mods[:, 4]], -1)
        m["modsel"] = np.ascontiguousarray(msel.transpose(3, 0, 1, 2, 4).reshape(128, L * 6 * KC * 2))
        mapsB.append(m)
    resB = run_bass_kernel_spmd(ncm, mapsB, core_ids=list(range(B)))
    out = np.stack([np.ascontiguousarray(resB.results[b]["outT"].T) for b in range(B)], 0)
    return out.astype(np.float32)


def kernel(**inputs):
    return run(FULL, inputs)
```

```python
import numpy as np
from contextlib import ExitStack
import concourse.bass as bass
import concourse.mybir as mybir
from concourse.bass_utils import run_bass_kernel_spmd

F32 = mybir.dt.float32
BF16 = mybir.dt.bfloat16
AF = mybir.ActivationFunctionType
ALU = mybir.AluOpType
AX = mybir.AxisListType
EPS = 1e-6
NCORES = 8

FULL = dict(D=4096, TL=4096, TC=256, H=16, CW=2048, GW=64, E=16, FF=256, L=4, B=4)


def derive(c):
    c = dict(c)
    c["KC"] = c["D"] // 128
    c["TA"] = c["TL"] + c["TC"]
    c["NTL"] = c["TL"] // 128
    c["NTC"] = c["TC"] // 128
    c["NTA"] = c["TA"] // 128
    c["DNW"] = c["H"] * 128
    c["CT"] = c["CW"] // 128
    c["ROWS"] = c["TL"] // c["GW"]
    c["MIXW"] = c["DNW"] + c["CW"]
    c["KCM"] = c["MIXW"] // 128
    c["INW"] = 4 * c["DNW"] + 4 * c["H"] + 2 * c["CW"]
    c["OFF_Z"] = 3 * c["DNW"]
    c["OFF_A"] = 4 * c["DNW"]
    c["OFF_G"] = 4 * c["DNW"] + c["CW"]
    c["OFF_BA"] = 4 * c["DNW"] + 2 * c["CW"]
    c["NBIN"] = (c["INW"] + 127) // 128
    c["FT"] = c["FF"] // 128
    c["EF"] = c["E"] * c["FF"]
    c["KCE"] = c["EF"] // 128
    c["NSH"] = 6 * c["D"] // NCORES
    c["CAPL"] = 2 * c["TL"] // c["E"]
    c["CAPC"] = 2 * c["TC"] // c["E"]
    szs = [c["NBIN"] * 128 * c["D"], c["KC"] * 128 * c["MIXW"], 2 * c["KCE"] * 128 * c["D"],
           c["KC"] * 128 * c["EF"]]
    offs = [0]
    for s in szs:
        offs.append(offs[-1] + s)
    c["WOFF"] = offs
    c["WFLAT"] = (offs[-1] + 2047) // 2048 * 2048
    return c


class Sem:
    def __init__(self, h):
        self.h = h
        self.count = 0


class Tok:
    __slots__ = ("w", "r", "ds", "excl")

    def __init__(self, excl=False):
        self.w = {}
        self.r = {}
        self.ds = None
        self.excl = excl


class Eng:
    def __init__(self, name, eng, sem):
        self.name = name
        self.eng = eng
        self.sem = sem
        self.waited = {}


class KB:
    def __init__(self, cfg):
        self.c = cfg
        self.nc = bass.Bass("TRN2", target_bir_lowering=False)
        nc = self.nc
        self.gstack = ExitStack()
        self.pstack = None
        mk = lambda n: Sem(self.gstack.enter_context(nc.semaphore(n)))
        self.PE = Eng("pe", nc.tensor, mk("s_pe"))
        self.DVE = Eng("dve", nc.vector, mk("s_dve"))
        self.ACT = Eng("act", nc.scalar, mk("s_act"))
        self.POOL = Eng("pool", nc.gpsimd, mk("s_pool"))
        self.SP = Eng("sp", nc.sync, mk("s_sp"))
        self.engs = [self.PE, self.DVE, self.ACT, self.POOL, self.SP]
        self.dpool = [mk(f"s_d{i}") for i in range(40)]
        self.dfree = list(self.dpool)
        self.ptoks = []
        self.banks = []
        for i in range(8):
            t = self.gstack.enter_context(nc.psum_tensor(f"ps{i}", [128, 512], F32))
            self.banks.append((t, Tok(excl=True)))
        self.bi = 0
        self.nid = 0

    def sb(self, shape, dt=F32, glob=False):
        self.nid += 1
        st = self.gstack if glob else self.pstack
        return st.enter_context(self.nc.sbuf_tensor(f"t{self.nid}", list(shape), dt))

    def tok(self, glob=False):
        t = Tok()
        if not glob:
            self.ptoks.append(t)
        return t

    def bank(self):
        b = self.banks[self.bi % 8]
        self.bi += 1
        return b

    def begin(self):
        self.pstack = ExitStack()
        self.ptoks = []

    def end(self):
        self.barrier()
        for t in self.ptoks:
            if t.ds is not None:
                self.dfree.append(t.ds)
                t.ds = None
        self.ptoks = []
        self.pstack.close()
        self.pstack = None

    def _wait(self, E, need):
        for s, v in need.items():
            if E.waited.get(s, 0) < v:
                E.eng.wait_ge(s.h, v)
                E.waited[s] = v

    def op(self, E, fn, outs=(), ins=()):
        need = {}

        def add(d, skip_same):
            for s, v in d.items():
                if s is E.sem and (skip_same or E is self.PE):
                    continue
                if need.get(s, 0) < v:
                    need[s] = v
        for t in ins:
            add(t.w, False)
            if t.excl:
                add(t.r, True)
        for t in outs:
            add(t.r, False)
            add(t.w, True)
        self._wait(E, need)
        ins_ = fn(E.eng)
        E.sem.count += 1
        ins_.then_inc(E.sem.h, 1)
        v = E.sem.count
        for t in ins:
            t.r[E.sem] = v
        for t in outs:
            t.w = {E.sem: v}
            t.r = {}
        return ins_

    def dma(self, out_ap, in_ap, wtok=None, rtok=None, extra_r=()):
        Q = self.SP
        need = {}

        def add(d):
            for s, v in d.items():
                if need.get(s, 0) < v:
                    need[s] = v
        if rtok is not None:
            add(rtok.w)
        for t in extra_r:
            add(t.w)
        if wtok is not None:
            add(wtok.w)
            add(wtok.r)
        self._wait(Q, need)
        t = wtok if wtok is not None else rtok
        if t.ds is None:
            t.ds = self.dfree.pop()
        ds = t.ds
        ds.count += 16
        Q.eng.dma_start(out=out_ap, in_=in_ap).then_inc(ds.h, 16)
        if wtok is not None:
            wtok.w = {ds: ds.count}
            wtok.r = {}
        if rtok is not None:
            rtok.r[ds] = ds.count

    def barrier(self):
        sems = [e.sem for e in self.engs] + self.dpool
        for E in self.engs:
            for s in sems:
                if s.count > 0 and E.waited.get(s, 0) < s.count:
                    E.eng.wait_ge(s.h, s.count)
                    E.waited[s] = s.count


def build(cfg):
    c = derive(cfg)
    kb = KB(c)
    nc = kb.nc
    PE, DVE, ACT, POOL = kb.PE, kb.DVE, kb.ACT, kb.POOL
    D, KC, TL, TC, TA, NTL, NTC, NTA = (c[k] for k in ("D", "KC", "TL", "TC", "TA", "NTL", "NTC", "NTA"))
    H, DNW, CW, CT, GW, ROWS, MIXW, KCM = (c[k] for k in ("H", "DNW", "CW", "CT", "GW", "ROWS", "MIXW", "KCM"))
    E_, FF, FT, EF, KCE, L, NSH, NBIN = (c[k] for k in ("E", "FF", "FT", "EF", "KCE", "L", "NSH", "NBIN"))
    WFLAT, WOFF = c["WFLAT"], c["WOFF"]

    def din(name, shape, dt=F32):
        return nc.dram_tensor(name, list(shape), dt, kind="ExternalInput").ap()

    def dint(name, shape, dt=F32):
        return nc.dram_tensor(name, list(shape), dt, kind="Internal").ap()

    xT_in = din("xT", [D, TA])
    condT2 = din("condT2", [D, 2])
    wada = din("wada", [L * D, 6 * D])
    bada = din("bada", [L, 6 * D])
    wall_in = din("wall", [L * (WFLAT // 2048), 2048])
    convw = din("convw", [128, L * 3 * H * 5])
    alog_b = din("alog_b", [128, L * 2 * H])
    dtb_b = din("dtb_b", [128, L * 2 * H])
    dnn = din("dnn", [128, L])
    cdw = din("cdw", [128, L * CT * 31])
    cvec = din("cvec", [128, L * 3 * CT])
    wrt = din("wrt", [128, L * KC * 16])
    fnorm = din("fnorm", [128, KC])
    consts = din("consts", [128, 6 * 128])
    selmat = din("selmat", [16, 16 * 128])
    outT = nc.dram_tensor("outT", [D, TL], F32, kind="ExternalOutput").ap()

    xT = dint("xres", [D, TA])
    hT = dint("hT", [NTA, 128, KC, 128], BF16)
    pT = dint("pT", [NBIN * 128, TA])
    qTd = dint("qTd", [H, 128, TA])
    kTd = dint("kTd", [H, 128, TA])
    ktokd = dint("ktokd", [H, NTA, 128, 128])
    vtokd = dint("vtokd", [H, NTA, 128, 128])
    catT = dint("catT", [NTA, 128, KCM, 128], BF16)
    yTd = dint("yTd", [CW, TA])
    actT = dint("actT", [NTA, 128, KCE, 128], BF16)
    lnst = dint("lnst", [2, 128, TA])

    def wview(l, idx, nb, kck):
        fl = wall_in[l * (WFLAT // 2048):(l + 1) * (WFLAT // 2048), :].rearrange("r c -> (r c)")
        return fl[WOFF[idx]:WOFF[idx] + nb * 128 * kck * 128].rearrange("(n k) -> n k", k=kck * 128)

    cst = kb.sb([128, 6 * 128], glob=True)
    cst_t = kb.tok(True)
    ident, ones, m_le, m_ge, m_lt, m_gt = (cst[:, i * 128:(i + 1) * 128] for i in range(6))
    selm = kb.sb([16, 16 * 128], glob=True)
    modT = kb.sb([128, L, 6, KC, 2], glob=True)
    mod_t = kb.tok(True)
    col = kb.sb([128, 4], glob=True)
    col_t = kb.tok(True)
    affT = kb.sb([16, TA], glob=True)
    aff_t = kb.tok(True)
    gwT = affT
    gw_t = aff_t
    smalls = kb.sb([128, L * 3 * H * 5 + 2 * L * 2 * H + L + L * CT * 31 + L * 3 * CT + L * KC * 16 + KC], glob=True)
    sm_t = kb.tok(True)
    o = 0
    convw_s = smalls[:, o:o + L * 3 * H * 5]; o += L * 3 * H * 5
    alog_s = smalls[:, o:o + L * 2 * H]; o += L * 2 * H
    dtb_s = smalls[:, o:o + L * 2 * H]; o += L * 2 * H
    dnn_s = smalls[:, o:o + L]; o += L
    cdw_s = smalls[:, o:o + L * CT * 31]; o += L * CT * 31
    cvec_s = smalls[:, o:o + L * 3 * CT]; o += L * 3 * CT
    wrt_s = smalls[:, o:o + L * KC * 16]; o += L * KC * 16
    fn_s = smalls[:, o:o + KC]; o += KC
    eps_c, one_c, zero_c = col[:, 0:1], col[:, 1:2], col[:, 2:3]

    kb.dma(cst[:], consts[:, :], wtok=cst_t)
    kb.dma(selm[:], selmat[:, :], wtok=cst_t)
    for dst, src in ((convw_s, convw), (alog_s, alog_b), (dtb_s, dtb_b), (dnn_s, dnn), (cdw_s, cdw),
                     (cvec_s, cvec), (wrt_s, wrt), (fn_s, fnorm)):
        kb.dma(dst, src[:, :], wtok=sm_t)
    kb.op(DVE, lambda e: e.memset(col[:, 0:1], EPS), outs=[col_t])
    kb.op(DVE, lambda e: e.memset(col[:, 1:2], 1.0), outs=[col_t])
    kb.op(DVE, lambda e: e.memset(col[:, 2:3], 0.0), outs=[col_t])
    kb.op(DVE, lambda e: e.memset(col[:, 3:4], 0.5), outs=[col_t])

    kb.begin()
    cp_t = kb.tok()
    xv_i = xT_in.rearrange("(a p) t -> p a t", p=128)
    xv_o = xT.rearrange("(a p) t -> p a t", p=128)
    for a in range(KC):
        kb.dma(xv_o[:, a, :], xv_i[:, a, :], wtok=cp_t)
    scT = kb.sb([128, KC, 2]); sc_t = kb.tok()
    kb.dma(scT[:], condT2.rearrange("(k p) i -> p k i", p=128), wtok=sc_t)
    kb.op(ACT, lambda e: e.activation(out=scT[:], in_=scT[:], func=AF.Silu), outs=[sc_t], ins=[sc_t])
    wts = [(kb.sb([128, KC, 512]), kb.tok()) for _ in range(2)]
    brows = [(kb.sb([1, 512]), kb.tok()) for _ in range(2)]
    mblks = [(kb.sb([2, 512]), kb.tok()) for _ in range(2)]
    NJ = 6 * D // 512
    for l in range(L):
        for j in range(NJ):
            wt, wt_t = wts[j % 2]
            br, br_t = brows[j % 2]
            mb, mb_t = mblks[j % 2]
            kb.dma(wt[:], wada[l * D:(l + 1) * D, j * 512:(j + 1) * 512].rearrange("(k p) n -> p k n", p=128), wtok=wt_t)
            kb.dma(br[:], bada[l:l + 1, j * 512:(j + 1) * 512], wtok=br_t)
            bt, bt_t = kb.bank()
            for kc in range(KC):
                kb.op(PE, lambda e, bt=bt, wt=wt, kc=kc: e.matmul(bt[0:2, 0:512], scT[:, kc, :], wt[:, kc, :],
                                                                  start=(kc == 0), stop=False), outs=[bt_t], ins=[sc_t, wt_t])
            kb.op(PE, lambda e, bt=bt, br=br: e.matmul(bt[0:2, 0:512], ones[0:1, 0:2], br[0:1, 0:512], start=False, stop=True),
                  outs=[bt_t], ins=[cst_t, br_t])
            kb.op(DVE, lambda e, bt=bt, mb=mb: e.tensor_copy(out=mb[:], in_=bt[0:2, 0:512]), outs=[mb_t], ins=[bt_t])
            b2_, b2_t = kb.bank()
            for k4 in range(4):
                kb.op(PE, lambda e, mb=mb, k4=k4: e.transpose(out=b2_[:, k4 * 2:k4 * 2 + 2], in_=mb[0:2, k4 * 128:(k4 + 1) * 128],
                                                              identity=ident[0:2, 0:2]), outs=[b2_t], ins=[mb_t, cst_t])
            colg = j * 512
            j6, kc0 = colg // D, (colg % D) // 128
            kb.op(DVE, lambda e, l=l, j6=j6, kc0=kc0: e.tensor_copy(
                out=modT[:, l, j6, kc0:kc0 + 4, :].rearrange("p k t -> p (k t)"), in_=b2_[:, 0:8]), outs=[mod_t], ins=[b2_t])
    for l in range(L):
        for j6 in (1, 4):
            kb.op(DVE, lambda e, l=l, j6=j6: e.tensor_scalar(
                out=modT[:, l, j6].rearrange("p k t -> p (k t)"), in0=modT[:, l, j6].rearrange("p k t -> p (k t)"),
                scalar1=1.0, scalar2=None, op0=ALU.add), outs=[mod_t], ins=[mod_t])
    kb.end()
    cc_t = [kb.tok(True) for _ in range(L + 1)]

    def mcol(l, j6, kc, is_ctx):
        return modT[:, l, j6, kc, (1 if is_ctx else 0):(2 if is_ctx else 1)]

    def norm_phase(scale_fn, shift_fn, dst_tiles=None, dst_f32=None, router_l=None, ntiles=NTA):
        kb.begin()
        xs = [(kb.sb([128, KC, 128]), kb.tok()) for _ in range(2)]
        sq = kb.sb([128, KC, 128]); sq_t = kb.tok()
        h32 = kb.sb([128, KC, 128]); h32_t = kb.tok()
        hbs = [(kb.sb([128, KC, 128], BF16), kb.tok()) for _ in range(2)]
        rs = kb.sb([128, 128]); rs_t = kb.tok()
        tmps = [(kb.sb([128, 128]), kb.tok()) for _ in range(2)]
        sm = kb.sb([128, 64]); sm_tk = kb.tok()
        xv = xT.rearrange("(k p) t -> p k t", p=128)
        for ti in range(ntiles):
            is_ctx = ti >= NTL
            x, x_t = xs[ti % 2]
            import os as _os
            KSUB = int(_os.environ.get("KSUB", "100"))
            kb.dma(x[:], xv[:, :, ti * 128:(ti + 1) * 128], wtok=x_t)
            if KSUB == 1: break
            kb.op(ACT, lambda e, x=x: e.square(out=sq[:], in_=x[:]), outs=[sq_t], ins=[x_t])
            if KSUB == 2: break
            bt, bt_t = kb.bank()
            for kc in range(KC):
                kb.op(PE, lambda e, bt=bt, kc=kc: e.matmul(bt[:, 0:128], ones, sq[:, kc, :], start=(kc == 0),
                                                            stop=(kc == KC - 1)), outs=[bt_t], ins=[sq_t, cst_t])
            if KSUB == 3: break
            kb.op(ACT, lambda e, bt=bt: e.activation(out=rs[:], in_=bt[:, 0:128], func=AF.Sqrt, bias=eps_c,
                                                     scale=1.0 / D), outs=[rs_t], ins=[bt_t, col_t])
            if KSUB == 4: break
            kb.op(DVE, lambda e: e.reciprocal(out=rs[:], in_=rs[:]), outs=[rs_t], ins=[rs_t])
            if KSUB == 5: break
            hb, hb_t = hbs[ti % 2]
            for kc in range(KC):
                tm, tm_t = tmps[kc % 2]
                kb.op(DVE, lambda e, tm=tm, x=x, kc=kc: e.tensor_tensor(out=tm[:], in0=x[:, kc, :], in1=rs[:],
                                                                         op=ALU.mult), outs=[tm_t], ins=[x_t, rs_t])
                sh = shift_fn(kc, is_ctx)
                kb.op(ACT, lambda e, tm=tm, kc=kc, sh=sh, is_ctx=is_ctx: e.activation(
                    out=h32[:, kc, :], in_=tm[:], func=AF.Identity, bias=(sh if sh is not None else zero_c),
                    scale=scale_fn(kc, is_ctx)), outs=[h32_t], ins=[tm_t, mod_t, sm_t, col_t])
            if KSUB == 6: break
            if dst_f32 is not None:
                kb.dma(dst_f32.rearrange("(k p) t -> p k t", p=128)[:, :, ti * 128:(ti + 1) * 128], h32[:], rtok=h32_t)
                continue
            kb.op(POOL, lambda e, hb=hb: e.tensor_copy(out=hb[:], in_=h32[:]), outs=[hb_t], ins=[h32_t])
            if KSUB == 7: break
            kb.dma(dst_tiles[ti], hb[:], rtok=hb_t)
            if KSUB == 8: break
            if router_l is not None:
                bt, bt_t = kb.bank()
                for kc in range(KC):
                    w0 = (router_l * KC + kc) * 16
                    kb.op(PE, lambda e, bt=bt, kc=kc, w0=w0: e.matmul(
                        bt[:, 0:16], h32[:, kc, :], wrt_s[:, w0:w0 + 16], start=(kc == 0), stop=(kc == KC - 1)),
                        outs=[bt_t], ins=[h32_t, sm_t])
                mx, nmx, ssum, ex, af = sm[:, 0:1], sm[:, 1:2], sm[:, 2:3], sm[:, 8:24], sm[:, 24:40]
                kb.op(DVE, lambda e, bt=bt: e.reduce_max(out=mx, in_=bt[:, 0:16], axis=AX.X), outs=[sm_tk], ins=[bt_t, sm_tk])
                kb.op(DVE, lambda e: e.tensor_scalar(out=nmx, in0=mx, scalar1=-1.0, scalar2=None, op0=ALU.mult),
                      outs=[sm_tk], ins=[sm_tk])
                kb.op(ACT, lambda e, bt=bt: e.activation(out=ex, in_=bt[:, 0:16], func=AF.Exp, bias=nmx, scale=1.0),
                      outs=[sm_tk], ins=[bt_t, sm_tk])
                kb.op(DVE, lambda e: e.reduce_sum(out=ssum, in_=ex, axis=AX.X), outs=[sm_tk], ins=[sm_tk])
                kb.op(DVE, lambda e: e.reciprocal(out=ssum, in_=ssum), outs=[sm_tk], ins=[sm_tk])
                kb.op(DVE, lambda e: e.tensor_scalar(out=af, in0=ex, scalar1=ssum, scalar2=None, op0=ALU.mult),
                      outs=[sm_tk], ins=[sm_tk])
                b2, b2_t = kb.bank()
                kb.op(PE, lambda e, b2=b2: e.transpose(out=b2[0:16, 0:128], in_=af, identity=ident),
                      outs=[b2_t], ins=[sm_tk, cst_t])
                kb.op(DVE, lambda e, b2=b2, ti=ti: e.tensor_copy(out=affT[:, ti * 128:(ti + 1) * 128], in_=b2[0:16, 0:128]),
                      outs=[aff_t], ins=[b2_t])
        kb.end()

    def groups_of(tiles):
        gs = []
        cur = []
        for t in tiles:
            if cur and (len(cur) == 4 or (t >= NTL) != (cur[0] >= NTL)):
                gs.append(cur); cur = []
            cur.append(t)
        if cur:
            gs.append(cur)
        return gs

    def supertiles(mxt=9):
        nst = (NTA + mxt - 1) // mxt
        base, rem = NTA // nst, NTA % nst
        sts = []
        t = 0
        for i in range(nst):
            n = base + (1 if i < rem else 0)
            sts.append(list(range(t, t + n)))
            t += n
        return sts

    def linear_phase(src, kck, wv, nb, cc_tok, epilogue, pair=False, pre_super=None):
        kb.begin()
        sts_ = supertiles(7 if pair else 9)
        mx_tiles = max(len(s) for s in sts_)
        S = kb.sb([128, mx_tiles, kck, 128], BF16); S_t = kb.tok()
        wfs = [(kb.sb([128, kck * 128]), kb.tok()) for _ in range(2)]
        wbs = [(kb.sb([128, kck, 128], BF16), kb.tok()) for _ in range(4 if pair else 2)]
        ctx_ = dict(kb=kb)
        if epilogue.setup:
            epilogue.setup(ctx_)
        wi = 0
        for st in sts_:
            t0, nt = st[0], len(st)
            kb.dma(S[:, 0:nt], src[t0:t0 + nt].rearrange("n p k t -> p n k t"), wtok=S_t)
            gs = groups_of(st)
            if pre_super:
                pre_super(ctx_, st, gs)
            step = 2 if pair else 1
            for b0 in range(0, nb, step):
                wbl = []
                for bb in range(step):
                    wf, wf_t = wfs[wi % 2]
                    wb, wb_t = wbs[wi % len(wbs)]
                    kb.dma(wf[:], wv[(b0 + bb) * 128:(b0 + bb + 1) * 128, :], wtok=wf_t, extra_r=[cc_tok])
                    ce = DVE if wi % 2 == 0 else POOL
                    kb.op(ce, lambda e, wb=wb, wf=wf: e.tensor_copy(out=wb[:].rearrange("p k n -> p (k n)"), in_=wf[:]),
                          outs=[wb_t], ins=[wf_t])
                    wbl.append((wb, wb_t))
                    wi += 1
                for g in gs:
                    g0 = g[0] - t0
                    ng = len(g)
                    bks = []
                    for (wb, wb_t) in wbl:
                        bt, bt_t = kb.bank()
                        for kc in range(kck):
                            kb.op(PE, lambda e, bt=bt, wb=wb, kc=kc, g0=g0, ng=ng: e.matmul(
                                bt[:, 0:ng * 128], wb[:, kc, :], S[:, g0:g0 + ng, kc, :], start=(kc == 0),
                                stop=(kc == kck - 1)), outs=[bt_t], ins=[wb_t, S_t])
                        bks.append((bt, bt_t))
                    epilogue(ctx_, b0, g, bks)
        kb.end()

    class Epi:
        def __init__(self, fn, setup=None):
            self.fn = fn
            self.setup = setup

        def __call__(self, *a):
            return self.fn(*a)

    def epi_store(dst):
        def setup(cx):
            cx["st"] = [(kb.sb([128, 512]), kb.tok()) for _ in range(3)]
            cx["i"] = 0

        def fn(cx, b0, g, bks):
            (bt, bt_t), = bks
            st, st_t = cx["st"][cx["i"] % 3]
            n = len(g) * 128
            eng = ACT if cx["i"] % 2 == 0 else DVE
            cx["i"] += 1
            if eng is ACT:
                kb.op(ACT, lambda e: e.activation(out=st[:, 0:n], in_=bt[:, 0:n], func=AF.Copy), outs=[st_t], ins=[bt_t])
            else:
                kb.op(DVE, lambda e: e.tensor_copy(out=st[:, 0:n], in_=bt[:, 0:n]), outs=[st_t], ins=[bt_t])
            kb.dma(dst[b0 * 128:(b0 + 1) * 128, g[0] * 128:g[0] * 128 + n], st[:, 0:n], rtok=st_t)
        return Epi(fn, setup)

    def epi_resid(l, j6):
        def setup(cx):
            cx["xo"] = [(kb.sb([128, 512]), kb.tok()) for _ in range(3)]
            cx["i"] = 0

        def fn(cx, b0, g, bks):
            (bt, bt_t), = bks
            xo, xo_t = cx["xo"][cx["i"] % 3]
            cx["i"] += 1
            n = len(g) * 128
            reg = xT[b0 * 128:(b0 + 1) * 128, g[0] * 128:g[0] * 128 + n]
            kb.dma(xo[:, 0:n], reg, wtok=xo_t)
            gc_ = mcol(l, j6, b0, g[0] >= NTL)
            kb.op(DVE, lambda e: e.scalar_tensor_tensor(out=xo[:, 0:n], in0=bt[:, 0:n], scalar=gc_, in1=xo[:, 0:n],
                                                        op0=ALU.mult, op1=ALU.add), outs=[xo_t], ins=[bt_t, xo_t, mod_t])
            kb.dma(reg, xo[:, 0:n], rtok=xo_t)
        return Epi(fn, setup)

    def epi_moe():
        def setup(cx):
            cx["gwb"] = kb.sb([128, 12 * 128]); cx["gwb_t"] = kb.tok()
            cx["sg"] = [(kb.sb([128, 512]), kb.tok()) for _ in range(2)]
            cx["ab"] = [(kb.sb([128, 512], BF16), kb.tok()) for _ in range(3)]
            cx["i"] = 0
            cx["cur_e"] = None

        def fn(cx, b0, g, bks):
            (ba_, ba_t), (bu, bu_t) = bks
            kidx = b0 // 2
            e_ = kidx // FT
            st = cx["st_tiles"]
            if cx["cur_e"] != (st[0], e_):
                cx["cur_e"] = (st[0], e_)
                for gg in cx["gs"]:
                    bt, bt_t = kb.bank()
                    n = len(gg) * 128
                    o0 = (gg[0] - st[0]) * 128
                    kb.op(PE, lambda e, bt=bt, n=n, gg=gg: e.matmul(
                        bt[:, 0:n], selm[0:16, e_ * 128:(e_ + 1) * 128], gwT[0:16, gg[0] * 128:gg[0] * 128 + n],
                        start=True, stop=True), outs=[bt_t], ins=[gw_t, cst_t])
                    kb.op(ACT, lambda e, bt=bt, n=n, o0=o0: e.activation(out=cx["gwb"][:, o0:o0 + n], in_=bt[:, 0:n],
                                                                          func=AF.Copy), outs=[cx["gwb_t"]], ins=[bt_t])
            n = len(g) * 128
            o0 = (g[0] - st[0]) * 128
            sg, sg_t = cx["sg"][cx["i"] % 2]
            ab, ab_t = cx["ab"][cx["i"] % 3]
            cx["i"] += 1
            kb.op(ACT, lambda e: e.activation(out=sg[:, 0:n], in_=ba_[:, 0:n], func=AF.Silu), outs=[sg_t], ins=[ba_t])
            kb.op(DVE, lambda e: e.tensor_tensor(out=sg[:, 0:n], in0=sg[:, 0:n], in1=bu[:, 0:n], op=ALU.mult),
                  outs=[sg_t], ins=[sg_t, bu_t])
            kb.op(POOL, lambda e: e.tensor_tensor(out=ab[:, 0:n], in0=sg[:, 0:n], in1=cx["gwb"][:, o0:o0 + n], op=ALU.mult),
                  outs=[ab_t], ins=[sg_t, cx["gwb_t"]])
            kb.dma(actT[g[0]:g[0] + len(g), :, kidx, :].rearrange("n p t -> p n t"),
                   ab[:, 0:n].rearrange("p (n t) -> p n t", t=128), rtok=ab_t)

        def pre(cx, st, gs):
            cx["st_tiles"] = st
            cx["gs"] = gs
        return Epi(fn, setup), pre

    def gates_phase(l, gate_b, gate_g, gt_t):
        kb.begin()
        baT = kb.sb([4 * H, TA]); ba_t = kb.tok()
        kb.dma(baT[:], pT[c["OFF_BA"]:c["OFF_BA"] + 4 * H, :], wtok=ba_t)
        raw = kb.sb([128, NTA, 4 * H]); raw_t = kb.tok()
        for ti in range(NTA):
            bt, bt_t = kb.bank()
            kb.op(PE, lambda e, bt=bt, ti=ti: e.transpose(out=bt[:, 0:4 * H], in_=baT[:, ti * 128:(ti + 1) * 128],
                                                           identity=ident[0:4 * H, 0:4 * H]), outs=[bt_t], ins=[ba_t, cst_t])
            kb.op(DVE, lambda e, bt=bt, ti=ti: e.tensor_copy(out=raw[:, ti, :], in_=bt[:, 0:4 * H]), outs=[raw_t], ins=[bt_t])
        nega = kb.sb([128, 2 * H]); ng_t = kb.tok()
        kb.op(ACT, lambda e: e.activation(out=nega[:], in_=alog_s[:, l * 2 * H:(l + 1) * 2 * H], func=AF.Exp),
              outs=[ng_t], ins=[sm_t])
        kb.op(DVE, lambda e: e.tensor_scalar(out=nega[:], in0=nega[:], scalar1=-1.0, scalar2=None, op0=ALU.mult),
              outs=[ng_t], ins=[ng_t])
        w1 = kb.sb([128, 2 * H]); w1_t = kb.tok()
        w2 = kb.sb([128, 2 * H]); w2_t = kb.tok()
        w3 = kb.sb([128, 2 * H]); w3_t = kb.tok()
        for ti in range(NTA):
            kb.op(ACT, lambda e, ti=ti: e.activation(out=w1[:], in_=raw[:, ti, 0:2 * H], func=AF.Exp, scale=-1.0),
                  outs=[w1_t], ins=[raw_t])
            kb.op(DVE, lambda e: e.tensor_scalar(out=w1[:], in0=w1[:], scalar1=1.0, scalar2=None, op0=ALU.add),
                  outs=[w1_t], ins=[w1_t])
            kb.op(DVE, lambda e, ti=ti: e.reciprocal(out=gate_b[:, ti, :], in_=w1[:]), outs=[gt_t], ins=[w1_t])
            kb.op(DVE, lambda e, ti=ti: e.tensor_tensor(out=w2[:], in0=raw[:, ti, 2 * H:4 * H],
                                                        in1=dtb_s[:, l * 2 * H:(l + 1) * 2 * H], op=ALU.add),
                  outs=[w2_t], ins=[raw_t, sm_t])
            kb.op(DVE, lambda e: e.tensor_scalar(out=w3[:], in0=w2[:], scalar1=-1.0, scalar2=None, op0=ALU.mult),
                  outs=[w3_t], ins=[w2_t])
            kb.op(DVE, lambda e: e.tensor_tensor(out=w3[:], in0=w3[:], in1=w2[:], op=ALU.max),
                  outs=[w3_t], ins=[w2_t, w3_t])
            kb.op(ACT, lambda e: e.activation(out=w3[:], in_=w3[:], func=AF.Exp, scale=-1.0), outs=[w3_t], ins=[w3_t])
            kb.op(ACT, lambda e: e.activation(out=w3[:], in_=w3[:], func=AF.Ln, bias=one_c, scale=1.0),
                  outs=[w3_t], ins=[w3_t, col_t])
            kb.op(DVE, lambda e: e.scalar_tensor_tensor(out=w3[:], in0=w2[:], scalar=0.0, in1=w3[:], op0=ALU.max,
                                                        op1=ALU.add), outs=[w3_t], ins=[w2_t, w3_t])
            kb.op(DVE, lambda e, ti=ti: e.tensor_tensor(out=gate_g[:, ti, :], in0=w3[:], in1=nega[:], op=ALU.mult),
                  outs=[gt_t], ins=[w3_t, ng_t])
        kb.end()

    def qkv_phase(l):
        kb.begin()
        PADW = TL + 4 + TC + 4
        xps = [(kb.sb([128, PADW]), kb.tok()) for _ in range(2)]
        for xp, xp_t in xps:
            kb.op(POOL, lambda e, xp=xp: e.memset(xp[:], 0.0), outs=[xp_t])
        accs = [(kb.sb([128, TA]), kb.tok()) for _ in range(2)]
        sq = kb.sb([128, TA]); sq_t = kb.tok()
        rn = kb.sb([128, 512]); rn_t = kb.tok()
        sts = [(kb.sb([128, 4, 128]), kb.tok()) for _ in range(2)]
        u = 0
        for comp in range(3):
            for h in range(H):
                xp, xp_t = xps[u % 2]
                acc, acc_t = accs[u % 2]
                ce = DVE
                u += 1
                r0 = comp * DNW + h * 128
                kb.dma(xp[:, 2:2 + TL], pT[r0:r0 + 128, 0:TL], wtok=xp_t)
                kb.dma(xp[:, TL + 6:TL + 6 + TC], pT[r0:r0 + 128, TL:TA], wtok=xp_t)
                import os as _os
                QS = int(_os.environ.get("QSUB", "100"))
                if QS == 1: break
                w0 = ((l * 3 + comp) * H + h) * 5
                for (o_out, o_in, n) in ((0, 0, TL), (TL, TL + 4, TC)):
                    for j in range(5):
                        wj = convw_s[:, w0 + j:w0 + j + 1]
                        if j == 0:
                            kb.op(ce, lambda e, wj=wj, o_out=o_out, o_in=o_in, n=n, j=j: e.tensor_scalar(
                                out=acc[:, o_out:o_out + n], in0=xp[:, o_in + j:o_in + j + n], scalar1=wj, scalar2=None,
                                op0=ALU.mult), outs=[acc_t], ins=[xp_t, sm_t])
                        else:
                            kb.op(ce, lambda e, wj=wj, o_out=o_out, o_in=o_in, n=n, j=j: e.scalar_tensor_tensor(
                                out=acc[:, o_out:o_out + n], in0=xp[:, o_in + j:o_in + j + n], scalar=wj,
                                in1=acc[:, o_out:o_out + n], op0=ALU.mult, op1=ALU.add), outs=[acc_t], ins=[xp_t, sm_t, acc_t])
                if QS == 2: break
                kb.op(ACT, lambda e: e.activation(out=acc[:], in_=acc[:], func=AF.Silu), outs=[acc_t], ins=[acc_t])
                if QS == 3: break
                if comp < 2:
                    kb.op(ACT, lambda e: e.activation(out=sq[:], in_=acc[:], func=AF.Square), outs=[sq_t], ins=[acc_t])
                    for t0 in range(0, TA, 512):
                        n = min(512, TA - t0)
                        bt, bt_t = kb.bank()
                        kb.op(PE, lambda e, bt=bt, t0=t0, n=n: e.matmul(bt[:, 0:n], ones, sq[:, t0:t0 + n], start=True,
                                                                         stop=True), outs=[bt_t], ins=[sq_t, cst_t])
                        kb.op(ACT, lambda e, bt=bt, n=n: e.activation(out=rn[:, 0:n], in_=bt[:, 0:n], func=AF.Sqrt,
                                                                      bias=eps_c, scale=1.0), outs=[rn_t], ins=[bt_t, col_t])
                        kb.op(DVE, lambda e, n=n: e.reciprocal(out=rn[:, 0:n], in_=rn[:, 0:n]), outs=[rn_t], ins=[rn_t])
                        sc_ = (128.0 ** -0.5) if comp == 0 else 1.0
                        kb.op(DVE, lambda e, t0=t0, n=n, sc_=sc_: e.scalar_tensor_tensor(
                            out=acc[:, t0:t0 + n], in0=acc[:, t0:t0 + n], scalar=sc_, in1=rn[:, 0:n], op0=ALU.mult,
                            op1=ALU.mult), outs=[acc_t], ins=[acc_t, rn_t])
                    if QS == 4: break
                    kb.dma((qTd if comp == 0 else kTd)[h], acc[:], rtok=acc_t)
                    if QS == 5: break
                if comp >= 1:
                    dst = ktokd if comp == 1 else vtokd
                    for gi, t0 in enumerate(range(0, NTA, 4)):
                        ng = min(4, NTA - t0)
                        bt, bt_t = kb.bank()
                        for k in range(ng):
                            kb.op(PE, lambda e, bt=bt, k=k, t0=t0: e.transpose(
                                out=bt[:, k * 128:(k + 1) * 128], in_=acc[:, (t0 + k) * 128:(t0 + k + 1) * 128],
                                identity=ident), outs=[bt_t], ins=[acc_t, cst_t])
                        st, st_t = sts[gi % 2]
                        kb.op(ACT if gi % 2 == 0 else DVE,
                              (lambda e, bt=bt, st=st, ng=ng: e.activation(out=st[:, 0:ng].rearrange("p n t -> p (n t)"),
                                                                           in_=bt[:, 0:ng * 128], func=AF.Copy))
                              if gi % 2 == 0 else
                              (lambda e, bt=bt, st=st, ng=ng: e.tensor_copy(out=st[:, 0:ng].rearrange("p n t -> p (n t)"),
                                                                            in_=bt[:, 0:ng * 128])),
                              outs=[st_t], ins=[bt_t])
                        kb.dma(dst[h, t0:t0 + ng].rearrange("n t d -> t n d"), st[:, 0:ng], rtok=st_t)
        kb.end()

    def deltanet_phase(l, gate_b, gate_g, gt_t):
        kb.begin()
        qf = kb.sb([128, TA]); qf_t = kb.tok()
        kf = kb.sb([128, TA]); kf_t = kb.tok()
        kt = kb.sb([128, NTA, 128]); kt_t = kb.tok()
        vt = kb.sb([128, NTA, 128]); vt_t = kb.tok()
        oacc = kb.sb([128, NTA, 128]); o_ts = [kb.tok() for _ in range(NTA)]
        oT = qf; oT_t = qf_t
        X1 = kf; X1_t = kf_t
        X2 = kt[:].rearrange("p n t -> p (n t)"); X2_t = kt_t
        cbs = [(kb.sb([128, 512], BF16), kb.tok()) for _ in range(2)]
        rnb = kb.sb([128, 512]); rnb_t = kb.tok()
        gcs = kb.sb([128, 2, 3, NTA]); gcs_t = kb.tok()
        Sst = [(kb.sb([128, 128]), kb.tok()) for _ in range(2)]
        NSLOT = 3
        names = ("E", "ET", "qd", "N", "M", "N2", "M2", "S", "TTw")
        alias = dict(Gb="E", Dn="E", Dt="ET", eR="qd", TTu="E", U="N", WT="M", kd="N2", vn="M2", qk="ET")
        slots = []
        for _d in range(2):
            row = []
            for _s in range(NSLOT):
                dct = {nm: (kb.sb([128, 128]), kb.tok()) for nm in names}
                for a_, b_ in alias.items():
                    dct[a_] = dct[b_]
                row.append(dct)
            slots.append(row)
        cols = [[(kb.sb([128, 4]), kb.tok()) for _ in range(NSLOT)] for _ in range(2)]
        order = [list(range(NTL, NTA)) + list(range(0, NTL)),
                 list(range(NTA - 1, NTL - 1, -1)) + list(range(NTL - 1, -1, -1))]
        dmask = [dict(cs=m_le, rr=m_le, am=m_gt, qm=m_le, last=127), dict(cs=m_ge, rr=m_ge, am=m_lt, qm=m_ge, last=0)]
        ee = [0]

        def alt():
            ee[0] += 1
            return DVE if ee[0] % 2 == 0 else POOL

        for h in range(H):
            kb.dma(qf[:], qTd[h], wtok=qf_t)
            kb.dma(kf[:], kTd[h], wtok=kf_t)
            kb.dma(kt[:], ktokd[h].rearrange("n t d -> t n d"), wtok=kt_t)
            kb.dma(vt[:], vtokd[h].rearrange("n t d -> t n d"), wtok=vt_t)
            for ti in range(NTA):
                kb.op(POOL, lambda e, ti=ti: e.memset(oacc[:, ti, :], 0.0), outs=[o_ts[ti]])
            for d in range(2):
                gi = d * H + h
                bt, bt_t = kb.bank()
                kb.op(PE, lambda e, bt=bt, d=d, gi=gi: e.matmul(bt[:, 0:NTA], dmask[d]["cs"], gate_g[:, :, gi],
                                                                 start=True, stop=True), outs=[bt_t], ins=[gt_t, cst_t])
                kb.op(DVE, lambda e, bt=bt, d=d: e.tensor_copy(out=gcs[:, d, 0, :], in_=bt[:, 0:NTA]), outs=[gcs_t], ins=[bt_t])
                kb.op(ACT, lambda e, d=d: e.activation(out=gcs[:, d, 1, :], in_=gcs[:, d, 0, :], func=AF.Exp),
                      outs=[gcs_t], ins=[gcs_t])
                kb.op(DVE, lambda e, d=d, gi=gi: e.tensor_tensor(out=gcs[:, d, 1, :], in0=gcs[:, d, 1, :],
                                                                 in1=gate_b[:, :, gi], op=ALU.mult),
                      outs=[gcs_t], ins=[gcs_t, gt_t])
                kb.op(DVE, lambda e, d=d, gi=gi: e.tensor_scalar(out=gcs[:, d, 2, :], in0=gate_b[:, :, gi], scalar1=-1.0,
                                                                 scalar2=None, op0=ALU.mult), outs=[gcs_t], ins=[gt_t])
                S0, S0_t = Sst[d]
                kb.op(POOL, lambda e, S0=S0: e.memset(S0[:], 0.0), outs=[S0_t])

            def pre(d, step):
                ci = order[d][step]
                sl = slots[d][step % NSLOT]
                cl, cl_t = cols[d][step % NSLOT]
                mk_ = dmask[d]
                gi = d * H + h
                g_col = gate_g[:, ci, gi:gi + 1]
                b_col = gate_b[:, ci, gi:gi + 1]
                gc_col = gcs[:, d, 0, ci:ci + 1]
                bw_col = gcs[:, d, 1, ci:ci + 1]
                nb_col = gcs[:, d, 2, ci:ci + 1]
                kT_c = kf[:, ci * 128:(ci + 1) * 128]
                qT_c = qf[:, ci * 128:(ci + 1) * 128]
                T = lambda nm: sl[nm][0]
                K = lambda nm: sl[nm][1]
                import os as _os
                PS = int(_os.environ.get('PSUB', '100'))
                kb.op(alt(), lambda e: e.tensor_scalar(out=T("Gb")[:], in0=ones, scalar1=g_col, scalar2=None, op0=ALU.mult),
                      outs=[K("Gb")], ins=[cst_t, gt_t])
                bR, bR_t = kb.bank()
                kb.op(PE, lambda e: e.matmul(bR[:, 0:128], T("Gb")[:], mk_["rr"], start=True, stop=True),
                      outs=[bR_t], ins=[K("Gb"), cst_t])
                if PS == 1: return
                kb.op(DVE, lambda e: e.tensor_scalar(out=T("Dn")[:], in0=bR[:, 0:128], scalar1=gc_col, scalar2=zero_c,
                                                     op0=ALU.subtract, op1=ALU.max), outs=[K("Dn")], ins=[bR_t, gcs_t])
                kb.op(ACT, lambda e: e.activation(out=T("E")[:], in_=T("Dn")[:], func=AF.Exp, scale=-1.0),
                      outs=[K("E")], ins=[K("Dn")])
                kb.op(DVE, lambda e: e.tensor_scalar(out=T("Dt")[:], in0=bR[:, 0:128], scalar1=gc_col, scalar2=zero_c,
                                                     op0=ALU.subtract, op1=ALU.min), outs=[K("Dt")], ins=[bR_t, gcs_t])
                kb.op(ACT, lambda e: e.activation(out=T("ET")[:], in_=T("Dt")[:], func=AF.Exp), outs=[K("ET")], ins=[K("Dt")])
                if PS == 2: return
                lc = mk_["last"]
                kb.op(ACT, lambda e: e.activation(out=cl[:, 0:1], in_=bR[:, lc:lc + 1], func=AF.Exp), outs=[cl_t], ins=[bR_t])
                kb.op(DVE, lambda e: e.tensor_tensor(out=cl[:, 1:2], in0=bR[:, lc:lc + 1], in1=gc_col, op=ALU.subtract),
                      outs=[cl_t], ins=[bR_t, gcs_t])
                kb.op(ACT, lambda e: e.activation(out=cl[:, 1:2], in_=cl[:, 1:2], func=AF.Exp), outs=[cl_t], ins=[cl_t])
                kb.op(ACT, lambda e: e.activation(out=T("eR")[:], in_=bR[:, 0:128], func=AF.Exp), outs=[K("eR")], ins=[bR_t])
                kb.op(alt(), lambda e: e.tensor_tensor(out=T("qd")[:], in0=qT_c, in1=T("eR")[:], op=ALU.mult),
                      outs=[K("qd")], ins=[qf_t, K("eR")])
                if PS == 3: return
                bK, bK_t = kb.bank()
                kb.op(PE, lambda e: e.matmul(bK[:, 0:128], kT_c, kT_c, start=True, stop=True), outs=[bK_t], ins=[kf_t])
                kb.op(POOL, lambda e: e.tensor_tensor(out=T("E")[:], in0=T("E")[:], in1=mk_["am"], op=ALU.mult),
                      outs=[K("E")], ins=[K("E"), cst_t])
                kb.op(DVE, lambda e: e.scalar_tensor_tensor(out=T("N")[:], in0=bK[:, 0:128], scalar=nb_col, in1=T("E")[:],
                                                            op0=ALU.mult, op1=ALU.mult), outs=[K("N")], ins=[bK_t, K("E"), gcs_t])
                if PS == 4: return
                bM, bM_t = kb.bank()
                kb.op(PE, lambda e: e.transpose(out=bM[:, 0:128], in_=T("N")[:], identity=ident), outs=[bM_t], ins=[K("N"), cst_t])
                kb.op(ACT, lambda e: e.activation(out=T("M")[:], in_=bM[:, 0:128], func=AF.Copy), outs=[K("M")], ins=[bM_t])
                kb.op(DVE, lambda e: e.tensor_tensor(out=T("S")[:], in0=bM[:, 0:128], in1=ident, op=ALU.add),
                      outs=[K("S")], ins=[bM_t, cst_t])
                if PS == 5: return
                pN, pM = "N", "M"
                for lev in range(1, 7):
                    nN = "N2" if pN == "N" else "N"
                    nM = "M2" if pM == "M" else "M"
                    bn, bn_t = kb.bank()
                    kb.op(PE, lambda e, bn=bn, pN=pN, pM=pM: e.matmul(bn[:, 0:128], T(pM)[:], T(pN)[:], start=True, stop=True),
                          outs=[bn_t], ins=[K(pM), K(pN)])
                    if lev < 6:
                        bm, bm_t = kb.bank()
                        kb.op(PE, lambda e, bm=bm, pN=pN, pM=pM: e.matmul(bm[:, 0:128], T(pN)[:], T(pM)[:], start=True, stop=True),
                              outs=[bm_t], ins=[K(pM), K(pN)])
                    kb.op(ACT, lambda e, bn=bn, nN=nN: e.activation(out=T(nN)[:], in_=bn[:, 0:128], func=AF.Copy),
                          outs=[K(nN)], ins=[bn_t])
                    if lev < 6:
                        kb.op(DVE, lambda e, bm=bm, nM=nM: e.tensor_copy(out=T(nM)[:], in_=bm[:, 0:128]), outs=[K(nM)], ins=[bm_t])
                    bs, bs_t = kb.bank()
                    kb.op(PE, lambda e, bs=bs, nN=nN: e.matmul(bs[:, 0:128], T(nN)[:], T("S")[:], start=True, stop=True),
                          outs=[bs_t], ins=[K(nN), K("S")])
                    kb.op(DVE, lambda e, bs=bs: e.tensor_tensor(out=T("S")[:], in0=bs[:, 0:128], in1=T("S")[:], op=ALU.add),
                          outs=[K("S")], ins=[bs_t, K("S")])
                    pN, pM = nN, nM
                if PS == 6: return
                kb.op(ACT, lambda e: e.activation(out=T("TTu")[:], in_=T("S")[:], func=AF.Copy, scale=b_col),
                      outs=[K("TTu")], ins=[K("S"), gt_t])
                kb.op(POOL, lambda e: e.tensor_scalar(out=T("TTw")[:], in0=T("S")[:], scalar1=bw_col, scalar2=None, op0=ALU.mult),
                      outs=[K("TTw")], ins=[K("S"), gcs_t])
                bU, bU_t = kb.bank()
                kb.op(PE, lambda e: e.matmul(bU[:, 0:128], T("TTu")[:], vt[:, ci, :], start=True, stop=True),
                      outs=[bU_t], ins=[K("TTu"), vt_t])
                kb.op(ACT, lambda e: e.activation(out=T("U")[:], in_=bU[:, 0:128], func=AF.Copy), outs=[K("U")], ins=[bU_t])
                bW, bW_t = kb.bank()
                kb.op(PE, lambda e: e.matmul(bW[:, 0:128], kt[:, ci, :], T("TTw")[:], start=True, stop=True),
                      outs=[bW_t], ins=[K("TTw"), kt_t])
                kb.op(DVE, lambda e: e.tensor_copy(out=T("WT")[:], in_=bW[:, 0:128]), outs=[K("WT")], ins=[bW_t])
                if PS == 7: return
                bQ, bQ_t = kb.bank()
                kb.op(PE, lambda e: e.matmul(bQ[:, 0:128], kT_c, qT_c, start=True, stop=True), outs=[bQ_t], ins=[kf_t, qf_t])
                kb.op(POOL, lambda e: e.tensor_tensor(out=T("ET")[:], in0=T("ET")[:], in1=mk_["qm"], op=ALU.mult),
                      outs=[K("ET")], ins=[K("ET"), cst_t])
                kb.op(DVE, lambda e: e.tensor_tensor(out=T("qk")[:], in0=bQ[:, 0:128], in1=T("ET")[:], op=ALU.mult),
                      outs=[K("qk")], ins=[bQ_t, K("ET")])
                kb.op(POOL, lambda e: e.tensor_scalar(out=T("kd")[:], in0=kt[:, ci, :], scalar1=cl[:, 1:2], scalar2=None,
                                                      op0=ALU.mult), outs=[K("kd")], ins=[kt_t, cl_t])

            def seq(d, step):
                ci = order[d][step]
                sl = slots[d][step % NSLOT]
                cl, cl_t = cols[d][step % NSLOT]
                T = lambda nm: sl[nm][0]
                K = lambda nm: sl[nm][1]
                S_, S_t = Sst[d]
                b1, b1_t = kb.bank()
                kb.op(PE, lambda e: e.matmul(b1[:, 0:128], T("WT")[:], S_[:], start=True, stop=True),
                      outs=[b1_t], ins=[K("WT"), S_t])
                b2, b2_t = kb.bank()
                kb.op(PE, lambda e: e.matmul(b2[:, 0:128], T("qd")[:], S_[:], start=True, stop=False),
                      outs=[b2_t], ins=[K("qd"), S_t])
                kb.op(DVE, lambda e: e.tensor_tensor(out=T("vn")[:], in0=T("U")[:], in1=b1[:, 0:128], op=ALU.subtract),
                      outs=[K("vn")], ins=[K("U"), b1_t])
                kb.op(PE, lambda e: e.matmul(b2[:, 0:128], T("qk")[:], T("vn")[:], start=False, stop=True),
                      outs=[b2_t], ins=[K("qk"), K("vn")])
                b3, b3_t = kb.bank()
                kb.op(PE, lambda e: e.matmul(b3[:, 0:128], T("kd")[:], T("vn")[:], start=True, stop=True),
                      outs=[b3_t], ins=[K("kd"), K("vn")])
                kb.op(DVE, lambda e: e.scalar_tensor_tensor(out=S_[:], in0=S_[:], scalar=cl[:, 0:1], in1=b3[:, 0:128],
                                                            op0=ALU.mult, op1=ALU.add), outs=[S_t], ins=[S_t, b3_t, cl_t])
                kb.op(POOL if False else DVE, lambda e: e.tensor_tensor(out=oacc[:, ci, :], in0=b2[:, 0:128], in1=oacc[:, ci, :],
                                                                        op=ALU.add), outs=[o_ts[ci]], ins=[b2_t, o_ts[ci]])

            import os as _os
            DS = int(_os.environ.get("DSUB", "100"))
            if DS == 1: continue
            pre(0, 0)
            if DS == 2: continue
            pre(1, 0)
            if DS == 3: continue
            for step in range(NTA):
                if step + 1 < NTA:
                    pre(0, step + 1)
                seq(0, step)
                if step + 1 < NTA:
                    pre(1, step + 1)
                seq(1, step)
            if DS == 4: continue
            for t0 in range(0, NTA, 4):
                ng = min(4, NTA - t0)
                bt, bt_t = kb.bank()
                for k in range(ng):
                    kb.op(PE, lambda e, bt=bt, k=k, t0=t0: e.transpose(out=bt[:, k * 128:(k + 1) * 128], in_=oacc[:, t0 + k, :],
                                                                       identity=ident), outs=[bt_t], ins=[o_ts[t0 + k], cst_t])
                kb.op(ACT, lambda e, bt=bt, t0=t0, ng=ng: e.activation(out=oT[:, t0 * 128:(t0 + ng) * 128], in_=bt[:, 0:ng * 128],
                                                                       func=AF.Copy), outs=[oT_t], ins=[bt_t])
            kb.op(ACT, lambda e: e.activation(out=X1[:], in_=oT[:], func=AF.Square), outs=[X1_t], ins=[oT_t])
            r0 = c["OFF_Z"] + h * 128
            kb.dma(X2, pT[r0:r0 + 128, :], wtok=X2_t)
            kb.op(ACT, lambda e: e.activation(out=X2, in_=X2, func=AF.Silu), outs=[X2_t], ins=[X2_t])
            for gi2, t0 in enumerate(range(0, NTA, 4)):
                ng = min(4, NTA - t0)
                n = ng * 128
                a0 = t0 * 128
                bt, bt_t = kb.bank()
                kb.op(PE, lambda e, bt=bt, a0=a0, n=n: e.matmul(bt[:, 0:n], ones, X1[:, a0:a0 + n], start=True, stop=True),
                      outs=[bt_t], ins=[X1_t, cst_t])
                kb.op(ACT, lambda e, bt=bt, n=n: e.activation(out=rnb[:, 0:n], in_=bt[:, 0:n], func=AF.Sqrt, bias=eps_c,
                                                              scale=1.0 / 128.0), outs=[rnb_t], ins=[bt_t, col_t])
                kb.op(DVE, lambda e, n=n: e.reciprocal(out=rnb[:, 0:n], in_=rnb[:, 0:n]), outs=[rnb_t], ins=[rnb_t])
                kb.op(DVE, lambda e, a0=a0, n=n: e.scalar_tensor_tensor(out=rnb[:, 0:n], in0=oT[:, a0:a0 + n],
                                                                        scalar=dnn_s[:, l:l + 1], in1=rnb[:, 0:n], op0=ALU.mult,
                                                                        op1=ALU.mult), outs=[rnb_t], ins=[oT_t, rnb_t, sm_t])
                cb, cb_t = cbs[gi2 % 2]
                kb.op(POOL, lambda e, cb=cb, a0=a0, n=n: e.tensor_tensor(out=cb[:, 0:n], in0=rnb[:, 0:n], in1=X2[:, a0:a0 + n],
                                                                         op=ALU.mult), outs=[cb_t], ins=[rnb_t, X2_t])
                kb.dma(catT[t0:t0 + ng, :, h, :].rearrange("n p t -> p n t"), cb[:, 0:n].rearrange("p (n t) -> p n t", t=128),
                       rtok=cb_t)
        kb.end()

    def conformer_phase(l):
        kb.begin()
        als = [(kb.sb([128, ROWS, GW]), kb.sb([128, TC]), kb.tok()) for _ in range(1)]
        gls = [(kb.sb([128, ROWS, GW]), kb.sb([128, TC]), kb.tok()) for _ in range(1)]
        uls = [(kb.sb([128, ROWS, GW]), kb.sb([128, TC]), kb.tok()) for _ in range(1)]
        yls = [(kb.sb([128, ROWS, GW]), kb.sb([128, TC]), kb.tok()) for _ in range(1)]
        st1 = kb.sb([128, TA]); st2 = kb.sb([128, TA]); st_t = kb.tok()
        sqb = kb.sb([128, 512]); sqb_t = kb.tok()
        kb.op(DVE, lambda e: e.memset(st1[:], 0.0), outs=[st_t])
        kb.op(DVE, lambda e: e.memset(st2[:], 0.0), outs=[st_t])
        RB = 512 // GW
        for ct in range(CT):
            al, ac, a_t = als[0]
            gl, gc_, g_t = gls[0]
            ul, uc, u_t = uls[0]
            yl, yc, y_t = yls[0]
            ce = DVE
            ra = c["OFF_A"] + ct * 128
            rg = c["OFF_G"] + ct * 128
            kb.dma(al[:], pT[ra:ra + 128, 0:TL].rearrange("p (r c) -> p r c", c=GW), wtok=a_t)
            kb.dma(ac[:], pT[ra:ra + 128, TL:TA], wtok=a_t)
            kb.dma(gl[:], pT[rg:rg + 128, 0:TL].rearrange("p (r c) -> p r c", c=GW), wtok=g_t)
            kb.dma(gc_[:], pT[rg:rg + 128, TL:TA], wtok=g_t)
            kb.op(ACT, lambda e, gl=gl: e.activation(out=gl[:], in_=gl[:], func=AF.Sigmoid), outs=[g_t], ins=[g_t])
            kb.op(ACT, lambda e, gc_=gc_: e.activation(out=gc_[:], in_=gc_[:], func=AF.Sigmoid), outs=[g_t], ins=[g_t])
            kb.op(ce, lambda e, ul=ul, al=al, gl=gl: e.tensor_tensor(out=ul[:], in0=al[:], in1=gl[:], op=ALU.mult),
                  outs=[u_t], ins=[a_t, g_t])
            kb.op(ce, lambda e, uc=uc, ac=ac, gc_=gc_: e.tensor_tensor(out=uc[:], in0=ac[:], in1=gc_[:], op=ALU.mult),
                  outs=[u_t], ins=[a_t, g_t])
            w0 = (l * CT + ct) * 31
            wc = lambda j: cdw_s[:, w0 + j:w0 + j + 1]
            bcol = cvec_s[:, (l * 3 + 0) * CT + ct:(l * 3 + 0) * CT + ct + 1]
            kb.op(ce, lambda e, yl=yl, ul=ul: e.tensor_scalar(out=yl[:], in0=ul[:], scalar1=wc(15), scalar2=bcol, op0=ALU.mult,
                                                              op1=ALU.add), outs=[y_t], ins=[u_t, sm_t])
            kb.op(ce, lambda e, yc=yc, uc=uc: e.tensor_scalar(out=yc[:], in0=uc[:], scalar1=wc(15), scalar2=bcol, op0=ALU.mult,
                                                              op1=ALU.add), outs=[y_t], ins=[u_t, sm_t])
            for j in range(31):
                if j == 15:
                    continue
                s = j - 15
                if ct < CT // 2:
                    c0, c1 = max(0, -s), min(GW, GW - s)
                    if c1 > c0:
                        kb.op(ce, lambda e, yl=yl, ul=ul, c0=c0, c1=c1, s=s, j=j: e.scalar_tensor_tensor(
                            out=yl[:, :, c0:c1], in0=ul[:, :, c0 + s:c1 + s], scalar=wc(j), in1=yl[:, :, c0:c1], op0=ALU.mult,
                            op1=ALU.add), outs=[y_t], ins=[u_t, y_t, sm_t])
                else:
                    r0_, r1_ = max(0, -s), min(ROWS, ROWS - s)
                    if r1_ > r0_:
                        kb.op(ce, lambda e, yl=yl, ul=ul, r0_=r0_, r1_=r1_, s=s, j=j: e.scalar_tensor_tensor(
                            out=yl[:, r0_:r1_, :], in0=ul[:, r0_ + s:r1_ + s, :], scalar=wc(j), in1=yl[:, r0_:r1_, :],
                            op0=ALU.mult, op1=ALU.add), outs=[y_t], ins=[u_t, y_t, sm_t])
                t0_, t1_ = max(0, -s), min(TC, TC - s)
                if t1_ > t0_:
                    kb.op(ce, lambda e, yc=yc, uc=uc, t0_=t0_, t1_=t1_, s=s, j=j: e.scalar_tensor_tensor(
                        out=yc[:, t0_:t1_], in0=uc[:, t0_ + s:t1_ + s], scalar=wc(j), in1=yc[:, t0_:t1_], op0=ALU.mult,
                        op1=ALU.add), outs=[y_t], ins=[u_t, y_t, sm_t])
            blocks = [(yl[:, r:r + RB, :], r * GW, RB * GW, True) for r in range(0, ROWS, RB)]
            blocks += [(yc[:, t:min(TC, t + 512)], TL + t, min(512, TC - t), False) for t in range(0, TC, 512)]
            for (ap_, o0, n, is3) in blocks:
                sqv = sqb[:, 0:n].rearrange("p (r c) -> p r c", c=GW) if is3 else sqb[:, 0:n]
                kb.op(ACT, lambda e, ap_=ap_, sqv=sqv: e.activation(out=sqv, in_=ap_, func=AF.Square), outs=[sqb_t], ins=[y_t])
                b1, b1_t = kb.bank()
                kb.op(PE, lambda e, b1=b1, ap_=ap_, n=n: e.matmul(b1[:, 0:n], ones, ap_, start=True, stop=True),
                      outs=[b1_t], ins=[y_t, cst_t])
                b2, b2_t = kb.bank()
                kb.op(PE, lambda e, b2=b2, n=n: e.matmul(b2[:, 0:n], ones, sqb[:, 0:n], start=True, stop=True),
                      outs=[b2_t], ins=[sqb_t, cst_t])
                kb.op(DVE, lambda e, b1=b1, o0=o0, n=n: e.tensor_tensor(out=st1[:, o0:o0 + n], in0=b1[:, 0:n], in1=st1[:, o0:o0 + n],
                                                                        op=ALU.add), outs=[st_t], ins=[b1_t, st_t])
                kb.op(DVE, lambda e, b2=b2, o0=o0, n=n: e.tensor_tensor(out=st2[:, o0:o0 + n], in0=b2[:, 0:n], in1=st2[:, o0:o0 + n],
                                                                        op=ALU.add), outs=[st_t], ins=[b2_t, st_t])
            kb.dma(yTd[ct * 128:(ct + 1) * 128, 0:TL].rearrange("p (r c) -> p r c", c=GW), yl[:], rtok=y_t)
            kb.dma(yTd[ct * 128:(ct + 1) * 128, TL:TA], yc[:], rtok=y_t)
        mu = kb.sb([128, TA]); mu_t = kb.tok()
        kb.op(DVE, lambda e: e.tensor_scalar(out=mu[:], in0=st1[:], scalar1=1.0 / CW, scalar2=None, op0=ALU.mult),
              outs=[mu_t], ins=[st_t])
        kb.op(DVE, lambda e: e.tensor_scalar(out=st2[:], in0=st2[:], scalar1=1.0 / CW, scalar2=None, op0=ALU.mult),
              outs=[st_t], ins=[st_t])
        kb.op(DVE, lambda e: e.tensor_tensor(out=st1[:], in0=mu[:], in1=mu[:], op=ALU.mult), outs=[st_t], ins=[mu_t, st_t])
        kb.op(DVE, lambda e: e.tensor_tensor(out=st2[:], in0=st2[:], in1=st1[:], op=ALU.subtract), outs=[st_t], ins=[st_t])
        kb.op(DVE, lambda e: e.tensor_scalar(out=st2[:], in0=st2[:], scalar1=0.0, scalar2=None, op0=ALU.max),
              outs=[st_t], ins=[st_t])
        kb.op(ACT, lambda e: e.activation(out=st1[:], in_=st2[:], func=AF.Sqrt, bias=eps_c, scale=1.0), outs=[st_t],
              ins=[st_t, col_t])
        kb.op(DVE, lambda e: e.reciprocal(out=st1[:], in_=st1[:]), outs=[st_t], ins=[st_t])
        kb.op(DVE, lambda e: e.scalar_tensor_tensor(out=st2[:], in0=mu[:], scalar=-1.0, in1=st1[:], op0=ALU.mult, op1=ALU.mult),
              outs=[st_t], ins=[mu_t, st_t])
        kb.dma(lnst[0], st1[:], rtok=st_t)
        kb.dma(lnst[1], st2[:], rtok=st_t)
        kb.end()
        kb.begin()
        st1 = kb.sb([128, TA]); st2 = kb.sb([128, TA]); st_t = kb.tok()
        kb.dma(st1[:], lnst[0], wtok=st_t)
        kb.dma(st2[:], lnst[1], wtok=st_t)
        ys = [(kb.sb([128, TA]), kb.tok()) for _ in range(2)]
        czs = [(kb.sb([128, TA], BF16), kb.tok()) for _ in range(2)]
        for ct in range(CT):
            y, y_t = ys[ct % 2]
            cz, cz_t = czs[ct % 2]
            ce = DVE
            kb.dma(y[:], yTd[ct * 128:(ct + 1) * 128, :], wtok=y_t)
            kb.op(ce, lambda e, y=y: e.tensor_tensor(out=y[:], in0=y[:], in1=st1[:], op=ALU.mult), outs=[y_t], ins=[y_t, st_t])
            kb.op(ce, lambda e, y=y: e.tensor_tensor(out=y[:], in0=y[:], in1=st2[:], op=ALU.add), outs=[y_t], ins=[y_t, st_t])
            gcol = cvec_s[:, (l * 3 + 1) * CT + ct:(l * 3 + 1) * CT + ct + 1]
            bcol2 = cvec_s[:, (l * 3 + 2) * CT + ct:(l * 3 + 2) * CT + ct + 1]
            kb.op(ACT, lambda e, y=y, cz=cz, gcol=gcol, bcol2=bcol2: e.activation(out=cz[:], in_=y[:], func=AF.Silu, bias=bcol2,
                                                                                 scale=gcol), outs=[cz_t], ins=[y_t, sm_t])
            kb.dma(catT[:, :, H + ct, :].rearrange("n p t -> p n t"), cz[:].rearrange("p (n t) -> p n t", t=128), rtok=cz_t)
        kb.end()

    def topk_phase():
        kb.begin()
        cmp_ = kb.sb([16, TL]); cmp_t = kb.tok()
        sc = kb.sb([16, 8]); sc_t = kb.tok()
        lo, hi, mid, cnt, ge, d1 = (sc[:, i:i + 1] for i in range(6))
        for (a0, n, k) in ((0, TL, c["CAPL"]), (TL, TC, c["CAPC"])):
            kb.op(DVE, lambda e: e.memset(lo, 0.0), outs=[sc_t])
            kb.op(DVE, lambda e: e.memset(hi, 1.0), outs=[sc_t], ins=[sc_t])
            for it in range(32):
                kb.op(DVE, lambda e: e.tensor_tensor(out=mid, in0=lo, in1=hi, op=ALU.add), outs=[sc_t], ins=[sc_t])
                kb.op(DVE, lambda e: e.tensor_scalar(out=mid, in0=mid, scalar1=0.5, scalar2=None, op0=ALU.mult), outs=[sc_t], ins=[sc_t])
                kb.op(DVE, lambda e, a0=a0, n=n: e.tensor_scalar(out=cmp_[:, 0:n], in0=affT[:, a0:a0 + n], scalar1=mid, scalar2=None,
                                                                 op0=ALU.is_ge), outs=[cmp_t], ins=[aff_t, sc_t])
                kb.op(DVE, lambda e, n=n: e.reduce_sum(out=cnt, in_=cmp_[:, 0:n], axis=AX.X), outs=[sc_t], ins=[cmp_t, sc_t])
                kb.op(DVE, lambda e, k=k: e.tensor_scalar(out=ge, in0=cnt, scalar1=float(k) - 0.5, scalar2=None, op0=ALU.is_gt),
                      outs=[sc_t], ins=[sc_t])
                kb.op(DVE, lambda e: e.tensor_tensor(out=d1, in0=mid, in1=lo, op=ALU.subtract), outs=[sc_t], ins=[sc_t])
                kb.op(DVE, lambda e: e.scalar_tensor_tensor(out=lo, in0=d1, scalar=ge, in1=lo, op0=ALU.mult, op1=ALU.add),
                      outs=[sc_t], ins=[sc_t])
                kb.op(DVE, lambda e: e.tensor_tensor(out=d1, in0=hi, in1=mid, op=ALU.subtract), outs=[sc_t], ins=[sc_t])
                kb.op(DVE, lambda e: e.scalar_tensor_tensor(out=hi, in0=d1, scalar=ge, in1=mid, op0=ALU.mult, op1=ALU.add),
                      outs=[sc_t], ins=[sc_t])
            kb.op(DVE, lambda e, a0=a0, n=n: e.scalar_tensor_tensor(out=gwT[:, a0:a0 + n], in0=affT[:, a0:a0 + n], scalar=lo,
                                                                    in1=affT[:, a0:a0 + n], op0=ALU.is_ge, op1=ALU.mult),
                  outs=[gw_t], ins=[aff_t, sc_t])
        kb.end()

    gate_b = kb.sb([128, NTA, 2 * H], glob=True)
    gate_g = kb.sb([128, NTA, 2 * H], glob=True)
    gt_t = kb.tok(True)
    hT_tiles = [hT[i] for i in range(NTA)]

    import os as _os
    _stop = int(_os.environ.get("KSTOP", "100000"))
    steps = []
    for l in range(L):
        cct = cc_t[l + 1]
        steps.append(lambda l=l: norm_phase(lambda kc, ic, l=l: mcol(l, 1, kc, ic), lambda kc, ic, l=l: mcol(l, 0, kc, ic),
                                            dst_tiles=hT_tiles))
        steps.append(lambda l=l, cct=cct: linear_phase(hT, KC, wview(l, 0, NBIN, KC), NBIN, cct, epi_store(pT)))
        steps.append(lambda l=l: gates_phase(l, gate_b, gate_g, gt_t))
        steps.append(lambda l=l: qkv_phase(l))
        steps.append(lambda l=l: deltanet_phase(l, gate_b, gate_g, gt_t))
        steps.append(lambda l=l: conformer_phase(l))
        steps.append(lambda l=l, cct=cct: linear_phase(catT, KCM, wview(l, 1, KC, KCM), KC, cct, epi_resid(l, 2)))
        steps.append(lambda l=l: norm_phase(lambda kc, ic, l=l: mcol(l, 4, kc, ic), lambda kc, ic, l=l: mcol(l, 3, kc, ic),
                                            dst_tiles=hT_tiles, router_l=l))
        steps.append(lambda l=l: topk_phase())

        def _moe_up(l=l, cct=cct):
            em, pre = epi_moe()
            linear_phase(hT, KC, wview(l, 2, 2 * KCE, KC), 2 * KCE, cct, em, pair=True, pre_super=pre)
        steps.append(_moe_up)
        steps.append(lambda l=l, cct=cct: linear_phase(actT, KCE, wview(l, 3, KC, KCE), KC, cct, epi_resid(l, 5)))
    steps.append(lambda: norm_phase(lambda kc, ic: fn_s[:, kc:kc + 1], lambda kc, ic: None, dst_f32=outT, ntiles=NTL))
    for i, st in enumerate(steps):
        if i >= _stop:
            break
        st()
    kb.barrier()
    return nc, c


def blocks_layout(W, nb, kck):
    K, N = W.shape
    Wp = np.zeros((kck * 128, nb * 128), np.float32)
    Wp[:K, :N] = W
    return np.ascontiguousarray(Wp.reshape(kck, 128, nb, 128).transpose(2, 1, 0, 3).reshape(nb * 128, kck * 128))


def build_ada(cfg):
    c = derive(cfg)
    kb = KB(c)
    nc = kb.nc
    PE, DVE, ACT = kb.PE, kb.DVE, kb.ACT
    D, KC, L, NSH = c["D"], c["KC"], c["L"], c["NSH"]
    condT = nc.dram_tensor("condT", [D, 5], F32, kind="ExternalInput").ap()
    wada = nc.dram_tensor("wada", [L * KC * 128, NSH], F32, kind="ExternalInput").ap()
    bada = nc.dram_tensor("bada", [L, NSH], F32, kind="ExternalInput").ap()
    mods = nc.dram_tensor("mods", [L * 5, NSH], F32, kind="ExternalOutput").ap()
    kb.begin()
    onesr = kb.sb([1, 8]); on_t = kb.tok()
    kb.op(DVE, lambda e: e.memset(onesr[:], 1.0), outs=[on_t])
    scT = kb.sb([128, KC, 5]); sc_t = kb.tok()
    kb.dma(scT[:], condT.rearrange("(k p) i -> p k i", p=128), wtok=sc_t)
    kb.op(ACT, lambda e: e.activation(out=scT[:], in_=scT[:], func=AF.Silu), outs=[sc_t], ins=[sc_t])
    NBK = min(512, NSH)
    NB6 = NSH // NBK
    wts = [(kb.sb([128, NSH]), kb.tok()) for _ in range(2)]
    brow = kb.sb([1, NSH]); br_t = kb.tok()
    mrow = kb.sb([5, NSH]); mr_t = kb.tok()
    for l in range(L):
        kb.dma(brow[:], bada[l:l + 1, :], wtok=br_t)
        bks = [kb.bank() for _ in range(NB6)]
        for kc in range(KC):
            wt, wt_t = wts[(l * KC + kc) % 2]
            r0 = (l * KC + kc) * 128
            kb.dma(wt[:], wada[r0:r0 + 128, :], wtok=wt_t)
            for j in range(NB6):
                bt, bt_t = bks[j]
                kb.op(PE, lambda e, bt=bt, wt=wt, kc=kc, j=j: e.matmul(
                    bt[0:5, 0:NBK], scT[:, kc, :], wt[:, j * NBK:(j + 1) * NBK], start=(kc == 0), stop=False),
                    outs=[bt_t], ins=[sc_t, wt_t])
        for j in range(NB6):
            bt, bt_t = bks[j]
            kb.op(PE, lambda e, bt=bt, j=j: e.matmul(
                bt[0:5, 0:NBK], onesr[0:1, 0:5], brow[0:1, j * NBK:(j + 1) * NBK], start=False, stop=True),
                outs=[bt_t], ins=[on_t, br_t])
            kb.op(DVE, lambda e, bt=bt, j=j: e.tensor_copy(out=mrow[:, j * NBK:(j + 1) * NBK], in_=bt[0:5, 0:NBK]),
                  outs=[mr_t], ins=[bt_t])
        kb.dma(mods[l * 5:(l + 1) * 5, :], mrow[:], rtok=mr_t)
    kb.end()
    kb.barrier()
    return nc, c


def prep_shared(cfg, inp):
    c = derive(cfg)
    D, KC, TL, TC, TA, H, DNW, CW, CT, L, E_, FF, FT, EF, KCE, NSH, NBIN, KCM = (
        c[k] for k in ("D", "KC", "TL", "TC", "TA", "H", "DNW", "CW", "CT", "L", "E", "FF", "FT", "EF", "KCE", "NSH",
                       "NBIN", "KCM"))
    f = lambda a: np.asarray(a, np.float32)
    shared = {}
    shared["convw"] = np.ascontiguousarray(
        f(inp["conv_qkv"]).reshape(L, 5, 3 * H, 128).transpose(3, 0, 2, 1).reshape(128, L * 3 * H * 5))
    shared["alog_b"] = np.ascontiguousarray(np.broadcast_to(f(inp["a_log"]).reshape(1, L * 2 * H), (128, L * 2 * H)))
    shared["dtb_b"] = np.ascontiguousarray(np.broadcast_to(f(inp["dt_bias"]).reshape(1, L * 2 * H), (128, L * 2 * H)))
    shared["dnn"] = np.ascontiguousarray(f(inp["dn_norm"]).T)
    shared["cdw"] = np.ascontiguousarray(
        f(inp["conf_dw"]).reshape(L, 31, CT, 128).transpose(3, 0, 2, 1).reshape(128, L * CT * 31))
    cv = np.stack([f(inp["conf_dw_b"]), f(inp["conf_ln_g"]), f(inp["conf_ln_b"])], 1)
    shared["cvec"] = np.ascontiguousarray(cv.reshape(L, 3, CT, 128).transpose(3, 0, 1, 2).reshape(128, L * 3 * CT))
    shared["wrt"] = np.ascontiguousarray(
        f(inp["w_router"]).reshape(L, KC, 128, 16).transpose(2, 0, 1, 3).reshape(128, L * KC * 16))
    shared["fnorm"] = np.ascontiguousarray(f(inp["final_norm"]).reshape(KC, 128).T)
    p = np.arange(128)[:, None]
    q = np.arange(128)[None, :]
    shared["consts"] = np.concatenate([(p == q), np.ones((128, 128)), (p <= q), (p >= q), (p < q), (p > q)],
                                      1).astype(np.float32)
    sm = np.zeros((16, 16, 128), np.float32)
    for e in range(16):
        sm[e, e, :] = 1.0
    shared["selmat"] = sm.reshape(16, 16 * 128)
    perm = np.concatenate([np.arange(0, 4 * DNW), np.arange(c["INW"] - 2 * CW, c["INW"]),
                           np.arange(4 * DNW, 4 * DNW + 4 * H)])
    wall = np.zeros((L, c["WFLAT"]), np.float32)
    for l in range(L):
        parts = [blocks_layout(f(inp["w_in"][l])[:, perm], NBIN, KC),
                 blocks_layout(f(inp["w_out"][l]), KC, KCM)]
        wg = f(inp["w_gate_e"][l]); wu = f(inp["w_up_e"][l])
        W1 = np.stack([wg.reshape(E_, D, FT, 128), wu.reshape(E_, D, FT, 128)], 3)
        W1 = W1.transpose(1, 0, 2, 3, 4).reshape(D, E_ * FT * 2 * 128)
        parts.append(blocks_layout(W1, 2 * KCE, KC))
        parts.append(blocks_layout(f(inp["w_down_e"][l]).reshape(EF, D), KC, KCE))
        for i, pa in enumerate(parts):
            wall[l, c["WOFF"][i]:c["WOFF"][i] + pa.size] = pa.ravel()
        del parts, W1
    shared["wall"] = wall.reshape(L * (c["WFLAT"] // 2048), 2048)
    return shared


_CACHE = {}


def run(cfg, inp):
    key = tuple(sorted(cfg.items()))
    if key not in _CACHE:
        _CACHE[key] = build(cfg)
    ncm, c = _CACHE[key]
    f = lambda a: np.asarray(a, np.float32)
    D, KC, L, B = c["D"], c["KC"], c["L"], c["B"]
    shared = prep_shared(cfg, inp)
    shared["wada"] = f(inp["w_ada"]).reshape(L * D, 6 * D)
    shared["bada"] = f(inp["b_ada"]).reshape(L, 6 * D)
    maps = []
    for b in range(B):
        m = dict(shared)
        m["xT"] = np.ascontiguousarray(np.concatenate([f(inp["x"][b]).T, f(inp["ctx"][b]).T], 1))
        m["condT2"] = np.ascontiguousarray(np.stack([f(inp["c"][b]), f(inp["c_ctx"])], 1))
        maps.append(m)
    res = run_bass_kernel_spmd(ncm, maps, core_ids=list(range(B)))
    out = np.stack([np.ascontiguousarray(res.results[b]["outT"].T) for b in range(B)], 0)
    return out.astype(np.float32)


def kernel(**inputs):
    return run(FULL, inputs)
```
